# Optimizing a Trainium2 kernel written in Bass

```python
import jax, jax.numpy as jnp
from jax import lax
import numpy as np

D_MODEL = 1024
BATCH = 16
SEQ = 4096
DEPTH = 1

ATTN_HEADS = 8
ATTN_KV_HEADS = 2
ATTN_GROUP = ATTN_HEADS // ATTN_KV_HEADS
ATTN_HEAD_DIM = 64
ATTN_WIDTH = ATTN_HEADS * ATTN_HEAD_DIM
KV_WIDTH = ATTN_KV_HEADS * ATTN_HEAD_DIM
WINDOW = 128
ATTN_BLOCK = 128

HGRN_HEADS = 4
HGRN_DK = 128
HGRN_DV = 128
HGRN_WIDTH = HGRN_HEADS * HGRN_DK
HGRN_V_WIDTH = HGRN_HEADS * HGRN_DV
HGRN_CHUNK = 32

N_BRANCHES = 2
IN_WIDTH = ATTN_WIDTH + 2 * KV_WIDTH + 2 * HGRN_WIDTH + 2 * HGRN_V_WIDTH + N_BRANCHES * D_MODEL

PEER_HEADS = 8
PEER_N_KEYS = 128
PEER_N_EXPERTS = PEER_N_KEYS * PEER_N_KEYS
PEER_KEY_DIM = 128
PEER_TOPK = 16
PEER_TOKEN_BLOCK = 128

EPS = 1e-6
MASK_VALUE = -1e30

kernel_name = "hybrid_swa_hgrn2_peer_gated_block"


def rms_norm(x, g):
    xf = x.astype(jnp.float32)
    y = xf * lax.rsqrt(jnp.mean(xf * xf, axis=-1, keepdims=True) + EPS)
    return (y * g.astype(jnp.float32)).astype(x.dtype)


def alibi_slopes():
    return jnp.asarray(2.0 ** (-8.0 * np.arange(1, ATTN_HEADS + 1) / ATTN_HEADS), dtype=jnp.float32)


def split_combined(proj):
    sizes = (ATTN_WIDTH, KV_WIDTH, KV_WIDTH, HGRN_WIDTH, HGRN_WIDTH,
             HGRN_V_WIDTH, HGRN_V_WIDTH, D_MODEL, D_MODEL)
    offsets = np.cumsum(sizes)[:-1].tolist()
    return jnp.split(proj, offsets, axis=-1)


def sliding_window_attention(q, k, v, sinks):
    B, S = q.shape[0], q.shape[1]
    W = ATTN_BLOCK
    nb = S // W
    qb = q.reshape(B, nb, W, ATTN_KV_HEADS, ATTN_GROUP, ATTN_HEAD_DIM)

    def with_prev(t):
        tb = t.reshape(B, nb, W, ATTN_KV_HEADS, ATTN_HEAD_DIM)
        prev = jnp.concatenate([jnp.zeros_like(tb[:, :1]), tb[:, :-1]], axis=1)
        return jnp.concatenate([prev, tb], axis=2)

    kc, vc = with_prev(k), with_prev(v)
    scale = ATTN_HEAD_DIM ** -0.5
    s = jnp.einsum('bnqkgd,bnskd->bnkgqs', qb, kc,
                   preferred_element_type=jnp.float32) * scale
    qpos = jnp.arange(W)[:, None] + W
    kpos = jnp.arange(2 * W)[None, :]
    dist = qpos - kpos
    blk = jnp.arange(nb)[:, None, None]
    valid = (dist >= 0) & (dist < WINDOW) & (blk * W + kpos - W >= 0)
    slopes = alibi_slopes().reshape(ATTN_KV_HEADS, ATTN_GROUP)
    s = s - slopes[:, :, None, None] * dist.astype(jnp.float32)
    s = jnp.where(valid[None, :, None, None], s, MASK_VALUE)
    sink = jnp.broadcast_to(sinks.astype(jnp.float32).reshape(ATTN_KV_HEADS, ATTN_GROUP)[:, :, None, None],
                            s.shape[:-1] + (1,))
    p = jax.nn.softmax(jnp.concatenate([s, sink], axis=-1), axis=-1)[..., :-1]
    o = jnp.einsum('bnkgqs,bnskd->bnqkgd', p.astype(v.dtype), vc)
    return o.reshape(B, S, ATTN_WIDTH)


def hgrn2_recurrence(q, f_logit, i, lower_bound):
    B, S = q.shape[0], q.shape[1]
    C = HGRN_CHUNK
    nc = S // C
    f32 = jnp.float32
    f = lower_bound + (1.0 - lower_bound) * jax.nn.sigmoid(f_logit.astype(f32))
    k = 1.0 - f
    logf = jnp.log(f)

    def chunk(t):
        return t.reshape(B, nc, C, HGRN_HEADS, t.shape[-1])

    qc, kc, lc, vc = chunk(q.astype(f32)), chunk(k), chunk(logf), chunk(i.astype(f32))
    b = jnp.cumsum(lc, axis=2)
    q_dec = qc * jnp.exp(b)
    k_inv = kc * jnp.exp(-b)
    causal = jnp.tril(jnp.ones((C, C), dtype=bool))
    a = jnp.where(causal, jnp.einsum('bnthd,bnshd->bnhts', q_dec, k_inv), 0.0)
    o_intra = jnp.einsum('bnhts,bnshv->bnthv', a, vc)
    b_last = b[:, :, -1:]
    ds = jnp.einsum('bnshd,bnshv->bnhdv', kc * jnp.exp(b_last - b), vc)
    decay = jnp.exp(b_last[:, :, 0])

    def step(state, inp):
        dec, d = inp
        return dec[..., None] * state + d, state

    s0 = jnp.zeros((B, HGRN_HEADS, HGRN_DK, HGRN_DV), f32)
    _, s_prev = lax.scan(step, s0, (jnp.moveaxis(decay, 1, 0), jnp.moveaxis(ds, 1, 0)))
    s_prev = jnp.moveaxis(s_prev, 0, 1)
    o_inter = jnp.einsum('bnthd,bnhdv->bnthv', q_dec, s_prev)
    return (o_intra + o_inter).reshape(B, S, HGRN_HEADS, HGRN_DV)


def peer_layer(h, w_q, keys, u, v):
    B, S = h.shape[0], h.shape[1]
    q = (h @ w_q).reshape(B, S, PEER_HEADS, 2, PEER_KEY_DIM)
    sc = jnp.einsum('bshpd,phnd->bshpn', q, keys).astype(jnp.float32)
    v1, i1 = lax.top_k(sc[..., 0, :], PEER_TOPK)
    v2, i2 = lax.top_k(sc[..., 1, :], PEER_TOPK)
    cand = (v1[..., :, None] + v2[..., None, :]).reshape(B, S, PEER_HEADS, PEER_TOPK * PEER_TOPK)
    cidx = (i1[..., :, None] * PEER_N_KEYS + i2[..., None, :]).reshape(B, S, PEER_HEADS, PEER_TOPK * PEER_TOPK)
    best, pos = lax.top_k(cand, PEER_TOPK)
    idx = jnp.take_along_axis(cidx, pos, axis=-1)
    gates = jax.nn.softmax(best, axis=-1)
    T = B * S
    nblk = T // PEER_TOKEN_BLOCK
    hk = PEER_HEADS * PEER_TOPK
    hb = h.reshape(nblk, PEER_TOKEN_BLOCK, D_MODEL)
    ib = idx.reshape(nblk, PEER_TOKEN_BLOCK, hk)
    gb = gates.reshape(nblk, PEER_TOKEN_BLOCK, hk).astype(h.dtype)

    def expert_block(args):
        ht, it, gt = args
        ue = jnp.take(u, it, axis=0)
        act = jax.nn.gelu(jnp.einsum('tkd,td->tk', ue, ht), approximate=False)
        ve = jnp.take(v, it, axis=0)
        return jnp.einsum('tk,tkd->td', gt * act, ve)

    out = lax.map(expert_block, (hb, ib, gb))
    return out.reshape(B, S, D_MODEL)


def hybrid_layer(x, norm_mix_g, w_in, sinks, lower_bound, hgrn_norm_g, w_attn_proj,
                 w_hgrn_proj, w_out, norm_ffn_g, w_peer_q, peer_keys, peer_u, peer_v):
    B, S = x.shape[0], x.shape[1]
    h = rms_norm(x, norm_mix_g)
    aq, ak, av, hq, hf, hi, hg, ga, gh = split_combined(h @ w_in)
    y_attn = sliding_window_attention(aq.reshape(B, S, ATTN_HEADS, ATTN_HEAD_DIM),
                                      ak.reshape(B, S, ATTN_KV_HEADS, ATTN_HEAD_DIM),
                                      av.reshape(B, S, ATTN_KV_HEADS, ATTN_HEAD_DIM), sinks)
    o = hgrn2_recurrence(jax.nn.silu(hq).reshape(B, S, HGRN_HEADS, HGRN_DK),
                         hf.reshape(B, S, HGRN_HEADS, HGRN_DK),
                         hi.reshape(B, S, HGRN_HEADS, HGRN_DV), lower_bound)
    o = rms_norm(o, hgrn_norm_g).astype(x.dtype) * jax.nn.silu(hg.reshape(B, S, HGRN_HEADS, HGRN_DV))
    y_hgrn = o.reshape(B, S, HGRN_V_WIDTH)
    merged = jax.nn.sigmoid(ga) * (y_attn @ w_attn_proj) + jax.nn.sigmoid(gh) * (y_hgrn @ w_hgrn_proj)
    x = x + merged @ w_out
    x = x + peer_layer(rms_norm(x, norm_ffn_g), w_peer_q, peer_keys, peer_u, peer_v)
    return x


def setup_inputs(seed: int = 0) -> dict:
    key = jax.random.key(seed)
    ks = jax.random.split(key, 16)
    f32 = jnp.float32
    n = jax.random.normal
    return {
        "x": n(ks[0], (BATCH, SEQ, D_MODEL), f32),
        "norm_mix_g": 1.0 + 0.02 * n(ks[1], (DEPTH, D_MODEL), f32),
        "w_in": n(ks[2], (DEPTH, D_MODEL, IN_WIDTH), f32) * D_MODEL ** -0.5,
        "attn_sinks": n(ks[3], (DEPTH, ATTN_HEADS), f32),
        "hgrn_lb_logits": 0.1 * n(ks[4], (DEPTH + 1, HGRN_WIDTH), f32),
        "hgrn_norm_g": 1.0 + 0.02 * n(ks[5], (DEPTH, HGRN_DV), f32),
        "w_attn_proj": n(ks[6], (DEPTH, ATTN_WIDTH, D_MODEL), f32) * ATTN_WIDTH ** -0.5,
        "w_hgrn_proj": n(ks[7], (DEPTH, HGRN_V_WIDTH, D_MODEL), f32) * HGRN_V_WIDTH ** -0.5,
        "w_out": n(ks[8], (DEPTH, D_MODEL, D_MODEL), f32) * D_MODEL ** -0.5,
        "norm_ffn_g": 1.0 + 0.02 * n(ks[9], (DEPTH, D_MODEL), f32),
        "w_peer_q": n(ks[10], (DEPTH, D_MODEL, PEER_HEADS * 2 * PEER_KEY_DIM), f32) * D_MODEL ** -0.5,
        "peer_keys": n(ks[11], (DEPTH, 2, PEER_HEADS, PEER_N_KEYS, PEER_KEY_DIM), f32) * PEER_KEY_DIM ** -0.5,
        "peer_u": n(ks[12], (DEPTH, PEER_N_EXPERTS, D_MODEL), f32) * D_MODEL ** -0.5,
        "peer_v": n(ks[13], (DEPTH, PEER_N_EXPERTS, D_MODEL), f32) * PEER_HEADS ** -0.5,
        "norm_final_g": 1.0 + 0.02 * n(ks[14], (D_MODEL,), f32),
    }


def reference(x, norm_mix_g, w_in, attn_sinks, hgrn_lb_logits, hgrn_norm_g, w_attn_proj,
              w_hgrn_proj, w_out, norm_ffn_g, w_peer_q, peer_keys, peer_u, peer_v, norm_final_g):
    lower_bounds = jnp.cumsum(jax.nn.softmax(hgrn_lb_logits.astype(jnp.float32), axis=0), axis=0)
    for layer in range(DEPTH):
        x = hybrid_layer(x, norm_mix_g[layer], w_in[layer], attn_sinks[layer],
                         lower_bounds[layer].reshape(HGRN_HEADS, HGRN_DK), hgrn_norm_g[layer],
                         w_attn_proj[layer], w_hgrn_proj[layer], w_out[layer], norm_ffn_g[layer],
                         w_peer_q[layer], peer_keys[layer], peer_u[layer], peer_v[layer])
    return rms_norm(x, norm_final_g)
```

```python
import numpy as np
from contextlib import ExitStack
import concourse.bass as bass
import concourse.mybir as mybir
from concourse.bass_utils import run_bass_kernel_spmd

F32 = mybir.dt.float32
BF16 = mybir.dt.bfloat16
U32 = mybir.dt.uint32
AF = mybir.ActivationFunctionType
ALU = mybir.AluOpType
AX = mybir.AxisListType

D = 1024
SEQ = 4096
NCORES = 8
EPS = 1e-6
NPIECE = 19
import os
STAGE = float(os.environ.get('KSTAGE', '9'))


class Buf:
    __slots__ = ("name", "t", "writers", "readers", "dsem", "dcount", "psum")

    def __init__(self, name, t=None):
        self.psum = False
        self.name = name
        self.t = t
        self.writers = {}
        self.readers = {}
        self.dsem = None
        self.dcount = 0


class Sched:
    ENGS = ("pe", "act", "dve", "pool", "sp")

    def __init__(self, nc, es):
        self.nc = nc
        self.es = es
        self.ops = {e: [] for e in self.ENGS}
        self.seq = {e: 0 for e in self.ENGS}
        self.seen = {e: {} for e in self.ENGS}
        self.esem = {e: es.enter_context(nc.semaphore("s_" + e)) for e in self.ENGS}

    def sb(self, name, shape, dt):
        return Buf(name, self.es.enter_context(self.nc.sbuf_tensor("sb_" + name, shape, dt)))

    def ps(self, name, shape, dt):
        b = Buf(name, self.es.enter_context(self.nc.psum_tensor("ps_" + name, shape, dt)))
        b.psum = True
        return b

    def _deps(self, eng, reads, writes, appends):
        deps = {}

        def add(d):
            for k, v in d.items():
                if deps.get(k, 0) < v:
                    deps[k] = v

        for b in reads:
            add(b.writers)
            if b.psum:
                add(b.readers)
        for b in writes:
            add(b.writers)
            add(b.readers)
        for b in appends:
            add(b.readers)
        out = []
        seen = self.seen[eng]
        for k, v in deps.items():
            if k == eng and eng in ("pe", "sp"):
                continue
            if seen.get(k, 0) >= v:
                continue
            seen[k] = v
            out.append((k, v))
        return out

    def _sem(self, k):
        return self.esem[k] if isinstance(k, str) else k

    def op(self, eng, fn, reads=(), writes=(), appends=()):
        waits = self._deps(eng, reads, writes, appends)
        self.seq[eng] += 1
        n = self.seq[eng]
        for b in reads:
            if b.readers.get(eng, 0) < n:
                b.readers[eng] = n
        for b in writes:
            b.writers = {eng: n}
            b.readers = {}
        for b in appends:
            b.writers[eng] = n
        self.ops[eng].append((fn, waits, (eng, 1)))

    def dma(self, q, fn, dst, reads=(), append=False):
        if dst.dsem is None:
            dst.dsem = self.es.enter_context(self.nc.semaphore("d_" + dst.name))
        waits = self._deps(q, reads, () if append else (dst,), (dst,) if append else ())
        dst.dcount += 16
        for b in reads:
            b.readers[dst.dsem] = dst.dcount
        if append:
            dst.writers[dst.dsem] = dst.dcount
        else:
            dst.writers = {dst.dsem: dst.dcount}
            dst.readers = {}
        self.ops[q].append((fn, waits, (dst.dsem, 16)))

    def wait_all(self, eng, bufs):
        waits = self._deps(eng, bufs, (), ())
        self.ops[eng].append((None, waits, None))

    def emit(self):
        nc = self.nc
        with nc.Block() as block:
            def run(engname):
                def body(e):
                    for fn, waits, inc in self.ops[engname]:
                        for k, v in waits:
                            e.wait_ge(self._sem(k), v)
                        if fn is not None:
                            ins = fn(e)
                            if inc is not None:
                                ins.then_inc(self._sem(inc[0]), inc[1])
                return body
            block.tensor(run("pe"))
            block.scalar(run("act"))
            block.vector(run("dve"))
            block.gpsimd(run("pool"))
            block.sync(run("sp"))


def build_nc(nseq, nt):
    ntok = nseq * nt * 128
    nc = bass.Bass("TRN2", target_bir_lowering=False)

    def din(name, shape, dt=F32):
        return nc.dram_tensor(name, shape, dt, kind="ExternalInput").ap()

    x_d = din("x", [ntok, D])
    wall_d = din("wall", [NPIECE, 128, 4096])
    u_d = din("pu", [16384, D])
    v_d = din("pv", [16384, D])
    keysT_d = din("keysT", [128, 2048])
    gcols_d = din("gcols", [128, 32])
    sinks_d = din("sinks", [1, 8])
    gffn_d = din("gffn_bc", [128, D])
    gfin_d = din("gfin_bc", [128, D])
    tab_d = din("attn_tab", [128, 2048])
    cst_d = din("consts", [128, 1024])
    out_d = nc.dram_tensor("out", [ntok, D], F32, kind="ExternalOutput").ap()
    ws_d = nc.dram_tensor("ws_bf", [NPIECE, 128, 4096], BF16, kind="Internal").ap()
    uvb_d = nc.dram_tensor("uvb_bf", [16384, 2 * D], BF16, kind="Internal").ap()

    with ExitStack() as es:
        S = Sched(nc, es)
        sb, op, dma = S.sb, S.op, S.dma

        ring = [sb(f"ring{i}", [128, 4096], BF16) for i in range(4)]
        X = [sb(f"X{i}", [128, D], F32) for i in range(2)]
        GSb = [sb(f"GSb{i}", [128, 2 * D], BF16) for i in range(8)]
        DG = [sb(f"DG{i}", [128, 128], BF16) for i in range(4)]
        keysT = sb("keysT", [128, 2048], BF16)
        gcols = sb("gcols", [128, 32], F32)
        lbc = sb("lbc", [128, 4], F32)
        omlb = sb("omlb", [128, 4], F32)
        sinks = sb("sinks", [1, 8], F32)
        esrow = sb("esrow", [1, 8, 64], BF16)
        onesrow = sb("onesrow", [1, 128], BF16)
        ones_bf = sb("ones_bf", [128, 128], BF16)
        ident = sb("ident", [128, 128], BF16)
        gffn_bc = sb("gffn_bc", [128, D], F32)
        gfin_bc = sb("gfin_bc", [128, D], F32)
        tab = sb("tab", [128, 2048], F32)
        cst = sb("cst", [128, 1024], F32)
        ws = Buf("ws")
        uvbuf = Buf("uvbuf")
        outb = Buf("outb")

        Hb = sb("Hb", [128, D], BF16)
        H2b = sb("H2b", [128, D], BF16)
        hT = sb("hT", [128, D], BF16)
        h2T = sb("h2T", [128, D], BF16)
        ss = sb("ss", [128, 12], F32)
        qT = sb("qT", [128, 1024], BF16)
        KT = [sb(f"KT{i}", [128, 128], BF16) for i in range(2)]
        V = [sb(f"V{i}", [128, 128], BF16) for i in range(2)]
        qs = sb("qs", [128, 512], F32)
        fT = sb("fT", [128, 512], F32)
        logf = sb("logf", [128, 512], F32)
        kT = sb("kT", [128, 512], F32)
        bT = sb("bT", [128, 512], F32)
        eb = sb("eb", [128, 512], F32)
        enb = sb("enb", [128, 512], F32)
        qd = sb("qd", [128, 512], BF16)
        kinv = sb("kinv", [128, 512], BF16)
        kdecT = sb("kdecT", [128, 512], BF16)
        kdec = sb("kdec", [128, 512], BF16)
        VH = sb("VH", [128, 512], BF16)
        sgate = sb("sgate", [128, 512], BF16)
        sga = sb("sga", [128, D], BF16)
        sgh = sb("sgh", [128, D], BF16)
        ATm = sb("ATm", [128, 512], BF16)
        osq = sb("osq", [128, 512], BF16)
        rn = sb("rn", [128, 512], F32)
        tmp = sb("tmp", [128, 512], F32)
        yhT = sb("yhT", [128, 512], BF16)
        yT = sb("yT", [64, D], BF16)
        rz = sb("rz", [64, D], F32)
        pe32 = [sb(f"pe32_{i}", [128, 512], F32) for i in range(2)]
        PT = [sb(f"PT{i}", [128, 512], BF16) for i in range(4)]
        mT = sb("mT", [128, D], BF16)
        t1 = pe32[0]
        t2 = pe32[1]
        S32 = [sb(f"S32_{i}", [128, 128], F32) for i in range(4)]
        Sbf = [sb(f"Sbf_{i}", [128, 128], BF16) for i in range(4)]
        X2 = sb("X2", [128, D], F32)
        ACC = sb("ACC", [128, D], F32)
        H2g = sb("H2g", [128, D], F32)
        junk = sb("junk", [128, D], BF16)
        junkD = sb("junkD", [128, D], BF16)
        qTp = sb("qTp", [128, 2048], BF16)
        SC = sb("SC", [128, 2048], F32)
        SCR = sb("SCR", [128, 256], F32)
        V16 = sb("V16", [128, 256], F32)
        I16 = sb("I16", [128, 256], U32)
        I16f = sb("I16f", [128, 256], F32)
        BIG1 = sb("BIG1", [128, 2048], F32)
        BIG2 = SC
        B16 = sb("B16", [128, 128], F32)
        P16 = sb("P16", [128, 128], U32)
        posf = sb("posf", [128, 128], F32)
        k1f = sb("k1f", [128, 128], F32)
        k2f = sb("k2f", [128, 128], F32)
        i1s = sb("i1s", [128, 128], F32)
        i2s = sb("i2s", [128, 128], F32)
        idxf = sb("idxf", [128, 128], F32)
        IDX = sb("IDX", [128, 128], U32)
        E16 = sb("E16", [128, 128], F32)
        negm = sb("negm", [128, 8], F32)
        Z = sb("Z", [128, 8], F32)
        rZ = sb("rZ", [128, 8], F32)
        ACTV = sb("ACTV", [128, 128], F32)
        GL = sb("GL", [128, 128], F32)
        Wt = sb("Wt", [128, 128], F32)

        banks = [S.ps(f"bank{i}", [128, 512], F32) for i in range(8)]
        bstate = [0]

        def nb():
            b = banks[bstate[0] % 5]
            bstate[0] += 1
            return b

        identA = cst.t[:, 0:128]
        hmaskA = cst.t[:, 128:256]
        resetA = cst.t[:, 256:768]
        iotaA = cst.t[:, 768:784]
        thrA = cst.t[:, 784:800]

        dma("sp", lambda e: e.dma_start(out=gcols.t[:], in_=gcols_d), gcols)
        dma("sp", lambda e: e.dma_start(out=sinks.t[:], in_=sinks_d), sinks)
        dma("sp", lambda e: e.dma_start(out=gffn_bc.t[:], in_=gffn_d), gffn_bc)
        dma("sp", lambda e: e.dma_start(out=gfin_bc.t[:], in_=gfin_d), gfin_bc)
        dma("sp", lambda e: e.dma_start(out=tab.t[:], in_=tab_d), tab)
        dma("sp", lambda e: e.dma_start(out=cst.t[:], in_=cst_d), cst)
        op("dve", lambda e: e.tensor_copy(out=ident.t[:], in_=identA), reads=[cst], writes=[ident])
        op("pool", lambda e: e.memset(ones_bf.t[:], 1.0), writes=[ones_bf])
        op("pool", lambda e: e.memset(onesrow.t[:], 1.0), writes=[onesrow])
        op("pool", lambda e: e.memset(qT.t[:], 0.0), writes=[qT])
        op("dve", lambda e: e.tensor_tensor(out=lbc.t[:], in0=gcols.t[:, 16:20], in1=gcols.t[:, 20:24], op=ALU.subtract),
           reads=[gcols], writes=[lbc])
        op("act", lambda e: e.activation(out=lbc.t[:], in_=lbc.t[:], func=AF.Sigmoid), reads=[lbc], writes=[lbc])
        op("dve", lambda e: e.tensor_scalar(out=omlb.t[:], in0=lbc.t[:], scalar1=-1.0, scalar2=1.0, op0=ALU.mult, op1=ALU.add),
           reads=[lbc], writes=[omlb])
        op("act", lambda e: e.activation(out=sinks.t[:], in_=sinks.t[:], func=AF.Exp), reads=[sinks], writes=[sinks])
        op("dve", lambda e: e.tensor_copy(out=esrow.t[:], in_=sinks.t[:].unsqueeze(2).to_broadcast([1, 8, 64])),
           reads=[sinks], writes=[esrow])
        st32 = [X[0], X[1], X2, ACC]
        for q in range(2):
            st = st32[q]
            dma("sp", lambda e, st=st, q=q: e.dma_start(out=st.t[:], in_=keysT_d[:, q * 1024:(q + 1) * 1024]), st)
            op("dve", lambda e, st=st, q=q: e.tensor_copy(out=keysT.t[:, q * 1024:(q + 1) * 1024], in_=st.t[:]),
               reads=[st], appends=[keysT])

        pro = []
        it = 0
        for tbl_d, coff in ((u_d, 0), (v_d, D)):
            for r in range(128):
                def mk(it=it, tbl_d=tbl_d, coff=coff, r=r):
                    st = st32[it % 4]
                    sv = GSb[it % 8]

                    def load():
                        dma("sp", lambda e: e.dma_start(out=st.t[:], in_=tbl_d[r * 128:(r + 1) * 128, :]), st)

                    def work():
                        eng = ("dve", "pool", "act")[it % 3]
                        if eng == "act":
                            op("act", lambda e: e.activation(out=sv.t[:, 0:D], in_=st.t[:], func=AF.Copy), reads=[st], writes=[sv])
                        else:
                            op(eng, lambda e: e.tensor_copy(out=sv.t[:, 0:D], in_=st.t[:]), reads=[st], writes=[sv])
                        dma("sp", lambda e: e.dma_start(out=uvb_d[r * 128:(r + 1) * 128, coff:coff + D], in_=sv.t[:, 0:D]),
                            uvbuf, reads=[sv], append=True)
                    return load, work
                pro.append(mk())
                it += 1
        for p in range(NPIECE):
            for q in range(4):
                def mk(it=it, p=p, q=q):
                    st = st32[it % 4]
                    sv = GSb[it % 8]
                    svb = sv.t[:]

                    def load():
                        dma("sp", lambda e: e.dma_start(out=st.t[:], in_=wall_d[p, :, q * 1024:(q + 1) * 1024]), st)

                    def work():
                        for hf_ in range(2):
                            c = q * 2 + hf_
                            src = st.t[:, hf_ * 512:(hf_ + 1) * 512]
                            dst = svb[:, hf_ * 512:(hf_ + 1) * 512]
                            eng = "dve" if (it + hf_) % 2 == 0 else "pool"
                            if p < 10:
                                sc = gcols.t[:, c:c + 1]
                            elif p >= 15:
                                sc = gcols.t[:, 8 + c:9 + c]
                            else:
                                sc = None
                            if sc is not None:
                                op(eng, lambda e, src=src, dst=dst, sc=sc: e.tensor_scalar(out=dst, in0=src, scalar1=sc, scalar2=None, op0=ALU.mult),
                                   reads=[st, gcols], appends=[sv])
                            else:
                                op(eng, lambda e, src=src, dst=dst: e.tensor_copy(out=dst, in_=src), reads=[st], appends=[sv])
                        dma("sp", lambda e: e.dma_start(out=ws_d[p, :, q * 1024:(q + 1) * 1024], in_=svb[:, 0:1024]),
                            ws, reads=[sv], append=True)
                    return load, work
                pro.append(mk())
                it += 1
        LA = 3
        for i in range(min(LA, len(pro))):
            pro[i][0]()
        for i in range(len(pro)):
            if i + LA < len(pro):
                pro[i + LA][0]()
            pro[i][1]()

        total_uses = nseq * nt * NPIECE
        rstate = {"loaded": 0}

        def ring_fetch_upto(n):
            while rstate["loaded"] < min(n, total_uses):
                i = rstate["loaded"]
                slot = ring[i % 4]
                p = i % NPIECE
                dma("sp", lambda e, slot=slot, p=p: e.dma_start(out=slot.t[:], in_=ws_d[p, :, :]), slot, reads=[ws])
                rstate["loaded"] += 1

        use_ctr = [0]

        def next_piece():
            i = use_ctr[0]
            ring_fetch_upto(i + 1)
            use_ctr[0] += 1
            return ring[i % 4]

        def prefetch():
            ring_fetch_upto(use_ctr[0] + 4)

        def load_x(n):
            xb = X[n % 2]
            dma("sp", lambda e, xb=xb, n=n: e.dma_start(out=xb.t[:], in_=x_d[n * 128:(n + 1) * 128, :]), xb)

        def rmsnorm_stats(src, col, name_reads):
            op("act", lambda e: e.activation(out=junk.t[:], in_=src.t[:], func=AF.Square, accum_out=ss.t[:, col:col + 1]),
               reads=[src], writes=[junk], appends=[ss])
            op("act", lambda e: e.activation(out=ss.t[:, col + 1:col + 2], in_=ss.t[:, col:col + 1], func=AF.Sqrt, bias=EPS, scale=1.0 / D),
               reads=[ss], appends=[ss])
            op("dve", lambda e: e.reciprocal(out=ss.t[:, col + 2:col + 3], in_=ss.t[:, col + 1:col + 2]), reads=[ss], appends=[ss])
            return ss.t[:, col + 2:col + 3]

        def transpose8(src, dst):
            bk = nb()
            bkb = bk.t[:].bitcast(BF16)
            for c in range(8):
                op("pe", lambda e, c=c: e.transpose(out=bkb[:, c * 128:(c + 1) * 128], in_=src.t[:, c * 128:(c + 1) * 128], identity=ident.t[:]),
                   reads=[src, ident], appends=[bk])
            op("act", lambda e: e.activation(out=dst.t[:], in_=bkb[:, 0:1024], func=AF.Copy), reads=[bk], writes=[dst])

        def fm_group(wb, ncols128, evac):
            bk = nb()
            w3 = wb.t[:].rearrange("p (c n) -> p c n", c=8)
            for m in range(ncols128):
                for c in range(8):
                    op("pe", lambda e, m=m, c=c: e.matmul(bk.t[:, m * 128:(m + 1) * 128], lhsT=w3[:, c, m * 128:(m + 1) * 128],
                                                           rhs=hT.t[:, c * 128:(c + 1) * 128], start=(c == 0), stop=(c == 7)),
                       reads=[wb, hT], appends=[bk])
            prefetch()
            evac(bk)

        def tm_group(wb, col0, ncols, bk, bcol0):
            w3 = wb.t[:].rearrange("p (c n) -> p c n", c=8)
            for c in range(8):
                op("pe", lambda e, c=c: e.matmul(bk.t[:, bcol0:bcol0 + ncols], lhsT=hT.t[:, c * 128:(c + 1) * 128],
                                                  rhs=w3[:, c, col0:col0 + ncols], start=(c == 0), stop=(c == 7)),
                   reads=[wb, hT], appends=[bk])

        def dump(src, n):
            dma("pool", lambda e, n=n, src=src: e.dma_start(out=out_d[n * 128:(n + 1) * 128, :], in_=src.t[:]), outb, reads=[src], append=True)

        regc = {}

        def breg(e):
            if "r" not in regc:
                regc["r"] = e.to_reg(16383)
            return regc["r"]

        load_x(0)
        n = 0
        for s in range(nseq):
            for hh in range(4):
                op("pool", lambda e, hh=hh: e.memset(S32[hh].t[:], 0.0), writes=[S32[hh]])
                op("pool", lambda e, hh=hh: e.memset(Sbf[hh].t[:], 0.0), writes=[Sbf[hh]])
            for j in range(nt):
                if n + 1 < nseq * nt:
                    load_x(n + 1)
                Xc = X[n % 2]
                cur, prv = j % 2, (j + 1) % 2
                if STAGE == 0:
                    dump(Xc, n); n += 1; continue
                r1 = rmsnorm_stats(Xc, 0, None)
                op("dve", lambda e, Xc=Xc, r1=r1: e.tensor_scalar(out=Hb.t[:], in0=Xc.t[:], scalar1=r1, scalar2=None, op0=ALU.mult),
                   reads=[Xc, ss], writes=[Hb])
                if STAGE == 0.3:
                    dump(Xc, n); n += 1; continue
                transpose8(Hb, hT)
                if STAGE == 0.6:
                    dump(Xc, n); n += 1; continue
                wb = next_piece()
                def evq(bk):
                    op("act", lambda e: e.activation(out=qT.t[0:64, 0:512], in_=bk.t[0:64, :], func=AF.Copy), reads=[bk], appends=[qT])
                    op("act", lambda e: e.activation(out=qT.t[64:128, 512:1024], in_=bk.t[64:128, :], func=AF.Copy), reads=[bk], appends=[qT])
                fm_group(wb, 4, evq)
                if STAGE == 0.7:
                    dump(Xc, n); n += 1; continue
                wb = next_piece()
                bk = nb()
                w3 = wb.t[:].rearrange("p (c n) -> p c n", c=8)
                for c in range(8):
                    op("pe", lambda e, c=c, w3=w3, bk=bk: e.matmul(bk.t[:, 0:128], lhsT=w3[:, c, 0:128], rhs=hT.t[:, c * 128:(c + 1) * 128],
                                                                  start=(c == 0), stop=(c == 7)), reads=[wb, hT], appends=[bk])
                tm_group(wb, 128, 128, bk, 128)
                prefetch()
                op("act", lambda e, bk=bk, cur=cur: e.activation(out=KT[cur].t[:], in_=bk.t[:, 0:128], func=AF.Copy), reads=[bk], writes=[KT[cur]])
                op("dve", lambda e, bk=bk, cur=cur: e.tensor_copy(out=V[cur].t[:], in_=bk.t[:, 128:256]), reads=[bk], writes=[V[cur]])
                if STAGE == 0.8:
                    dump(Xc, n); n += 1; continue
                wb = next_piece()
                fm_group(wb, 4, lambda bk: op("act", lambda e: e.activation(out=qs.t[:], in_=bk.t[:], func=AF.Silu), reads=[bk], writes=[qs]))
                wb = next_piece()
                fm_group(wb, 4, lambda bk: op("act", lambda e: e.activation(out=fT.t[:], in_=bk.t[:], func=AF.Sigmoid), reads=[bk], writes=[fT]))
                if STAGE == 0.9:
                    dump(Xc, n); n += 1; continue
                wb = next_piece()
                bk = nb()
                tm_group(wb, 0, 512, bk, 0)
                prefetch()
                op("dve", lambda e, bk=bk: e.tensor_copy(out=VH.t[:], in_=bk.t[:]), reads=[bk], writes=[VH])
                wb = next_piece()
                fm_group(wb, 4, lambda bk: op("act", lambda e: e.activation(out=sgate.t[:], in_=bk.t[:], func=AF.Silu), reads=[bk], writes=[sgate]))
                for half in range(2):
                    wb = next_piece()
                    fm_group(wb, 4, lambda bk, half=half: op("act", lambda e: e.activation(out=sga.t[:, half * 512:(half + 1) * 512], in_=bk.t[:], func=AF.Sigmoid),
                                                             reads=[bk], appends=[sga]))
                for half in range(2):
                    wb = next_piece()
                    fm_group(wb, 4, lambda bk, half=half: op("act", lambda e: e.activation(out=sgh.t[:, half * 512:(half + 1) * 512], in_=bk.t[:], func=AF.Sigmoid),
                                                             reads=[bk], appends=[sgh]))

                if STAGE == 1:
                    dump(Xc, n); n += 1; continue
                blks = [1] if j == 0 else [0, 1]
                lo = 256 if j == 0 else 0
                for b in range(4):
                    bk = nb()
                    for blk in blks:
                        Kb = KT[prv] if blk == 0 else KT[cur]
                        for hl in range(2):
                            col = (blk * 2 + hl) * 128
                            hq0 = (b if hl == 0 else 4 + b) * 128
                            op("pe", lambda e, bk=bk, Kb=Kb, col=col, hq0=hq0: e.matmul(
                                bk.t[:, col:col + 128], lhsT=Kb.t[:, :], rhs=qT.t[:, hq0:hq0 + 128], start=True, stop=True),
                               reads=[Kb, qT], appends=[bk])
                    pe_ = pe32[b % 2]
                    op("act", lambda e, bk=bk, pe_=pe_, lo=lo: e.activation(out=pe_.t[:, lo:512], in_=bk.t[:, lo:512], func=AF.Exp, scale=0.125),
                       reads=[bk], writes=[pe_])
                    op("dve", lambda e, pe_=pe_, b=b, lo=lo: e.tensor_tensor(out=PT[b].t[:, lo:512], in0=pe_.t[:, lo:512],
                                                                             in1=tab.t[:, b * 512 + lo:(b + 1) * 512], op=ALU.mult),
                       reads=[pe_, tab], writes=[PT[b]])
                for g in range(2):
                    po = nb()
                    pz = nb()
                    for hq_ in range(4):
                        hh = g * 4 + hq_
                        b, hl = (hh, 0) if hh < 4 else (hh - 4, 1)
                        c0 = hq_ * 128
                        for bi, blk in enumerate(blks):
                            Vb = V[prv] if blk == 0 else V[cur]
                            col = (blk * 2 + hl) * 128
                            last = (bi == len(blks) - 1)
                            op("pe", lambda e, po=po, Vb=Vb, hl=hl, b=b, col=col, c0=c0, bi=bi, last=last: e.matmul(
                                po.t[0:64, c0:c0 + 128], lhsT=Vb.t[:, hl * 64:(hl + 1) * 64], rhs=PT[b].t[:, col:col + 128],
                                start=(bi == 0), stop=last), reads=[Vb, PT[b]], appends=[po])
                        for bi, blk in enumerate(blks):
                            col = (blk * 2 + hl) * 128
                            op("pe", lambda e, pz=pz, b=b, col=col, c0=c0, bi=bi: e.matmul(
                                pz.t[0:64, c0:c0 + 128], lhsT=ones_bf.t[:, 0:64], rhs=PT[b].t[:, col:col + 128],
                                start=(bi == 0), stop=False), reads=[ones_bf, PT[b]], appends=[pz])
                        op("pe", lambda e, pz=pz, hh=hh, c0=c0: e.matmul(
                            pz.t[0:64, c0:c0 + 128], lhsT=esrow.t[0:1, hh, :], rhs=onesrow.t[0:1, :], start=False, stop=True),
                           reads=[esrow, onesrow], appends=[pz])
                    op("dve", lambda e, pz=pz, g=g: e.reciprocal(out=rz.t[:, g * 512:(g + 1) * 512], in_=pz.t[0:64, :]), reads=[pz], appends=[rz])
                    op("dve", lambda e, po=po, g=g: e.tensor_tensor(out=yT.t[:, g * 512:(g + 1) * 512], in0=po.t[0:64, :],
                                                                    in1=rz.t[:, g * 512:(g + 1) * 512], op=ALU.mult),
                       reads=[po, rz], appends=[yT])

                if STAGE == 2:
                    dump(Xc, n); n += 1; continue
                for hh in range(4):
                    op("dve", lambda e, hh=hh: e.tensor_scalar(out=fT.t[:, hh * 128:(hh + 1) * 128], in0=fT.t[:, hh * 128:(hh + 1) * 128],
                                                               scalar1=omlb.t[:, hh:hh + 1], scalar2=lbc.t[:, hh:hh + 1], op0=ALU.mult, op1=ALU.add),
                       reads=[fT, omlb, lbc], appends=[fT])
                op("act", lambda e: e.activation(out=logf.t[:], in_=fT.t[:], func=AF.Ln), reads=[fT], writes=[logf])
                op("dve", lambda e: e.tensor_scalar(out=kT.t[:], in0=fT.t[:], scalar1=-1.0, scalar2=1.0, op0=ALU.mult, op1=ALU.add),
                   reads=[fT], writes=[kT])
                op("dve", lambda e: e.tensor_tensor_scan(out=bT.t[:], data0=resetA, data1=logf.t[:], initial=0.0, op0=ALU.mult, op1=ALU.add),
                   reads=[cst, logf], writes=[bT])
                op("act", lambda e: e.activation(out=eb.t[:], in_=bT.t[:], func=AF.Exp), reads=[bT], writes=[eb])
                op("act", lambda e: e.activation(out=enb.t[:], in_=bT.t[:], func=AF.Exp, scale=-1.0), reads=[bT], writes=[enb])
                op("dve", lambda e: e.tensor_tensor(out=qd.t[:], in0=qs.t[:], in1=eb.t[:], op=ALU.mult), reads=[qs, eb], writes=[qd])
                op("dve", lambda e: e.tensor_tensor(out=kT.t[:], in0=kT.t[:], in1=enb.t[:], op=ALU.mult), reads=[kT, enb], writes=[kT])
                op("pool", lambda e: e.tensor_copy(out=kinv.t[:], in_=kT.t[:]), reads=[kT], writes=[kinv])
                for hh in range(4):
                    for ch in range(2):
                        c0 = hh * 128 + ch * 64
                        op("dve", lambda e, c0=c0: e.tensor_scalar(out=kdecT.t[:, c0:c0 + 64], in0=kT.t[:, c0:c0 + 64],
                                                                   scalar1=eb.t[:, c0 + 63:c0 + 64], scalar2=None, op0=ALU.mult),
                           reads=[kT, eb], appends=[kdecT])
                bkA = nb()
                for hh in range(4):
                    op("pe", lambda e, hh=hh, bkA=bkA: e.matmul(bkA.t[:, hh * 128:(hh + 1) * 128], lhsT=kinv.t[:, hh * 128:(hh + 1) * 128],
                                                                rhs=qd.t[:, hh * 128:(hh + 1) * 128], start=True, stop=True),
                       reads=[kinv, qd], appends=[bkA])
                op("dve", lambda e, bkA=bkA: e.tensor_tensor(out=ATm.t[:].rearrange("p (h t) -> p h t", h=4),
                                                             in0=bkA.t[:].rearrange("p (h t) -> p h t", h=4),
                                                             in1=hmaskA.unsqueeze(1).to_broadcast([128, 4, 128]), op=ALU.mult),
                   reads=[bkA, cst], writes=[ATm])
                bkK = nb()
                bkKb = bkK.t[:].bitcast(BF16)
                for hh in range(4):
                    op("pe", lambda e, hh=hh, bkKb=bkKb: e.transpose(out=bkKb[:, hh * 128:(hh + 1) * 128], in_=kdecT.t[:, hh * 128:(hh + 1) * 128],
                                                                     identity=ident.t[:]), reads=[kdecT, ident], appends=[bkK])
                op("act", lambda e, bkKb=bkKb: e.activation(out=kdec.t[:], in_=bkKb[:, 0:512], func=AF.Copy), reads=[bkK], writes=[kdec])
                bkO = banks[7]
                for hh in range(4):
                    c0 = hh * 128
                    op("pe", lambda e, c0=c0, hh=hh, bkO=bkO: e.matmul(bkO.t[:, c0:c0 + 128], lhsT=VH.t[:, c0:c0 + 128], rhs=ATm.t[:, c0:c0 + 128],
                                                                       start=True, stop=False), reads=[VH, ATm], appends=[bkO])
                    for ch in range(2):
                        cc = c0 + ch * 64
                        op("pe", lambda e, cc=cc, hh=hh, ch=ch, bkO=bkO: e.matmul(bkO.t[:, cc:cc + 64], lhsT=Sbf[hh].t[:], rhs=qd.t[:, cc:cc + 64],
                                                                                  start=False, stop=(ch == 1)), reads=[Sbf[hh], qd], appends=[bkO])
                        bkD = nb()
                        r0 = ch * 64
                        op("pe", lambda e, bkD=bkD, r0=r0, c0=c0: e.matmul(bkD.t[:, 0:128], lhsT=kdec.t[r0:r0 + 64, c0:c0 + 128],
                                                                           rhs=VH.t[r0:r0 + 64, c0:c0 + 128], start=True, stop=True),
                           reads=[kdec, VH], appends=[bkD])
                        op("dve", lambda e, bkD=bkD, hh=hh, cc=cc: e.scalar_tensor_tensor(out=S32[hh].t[:], in0=S32[hh].t[:], scalar=eb.t[:, cc + 63:cc + 64],
                                                                                          in1=bkD.t[:, 0:128], op0=ALU.mult, op1=ALU.add),
                           reads=[bkD, eb, S32[hh]], writes=[S32[hh]])
                        op("act", lambda e, hh=hh: e.activation(out=Sbf[hh].t[:], in_=S32[hh].t[:], func=AF.Copy), reads=[S32[hh]], writes=[Sbf[hh]])
                op("act", lambda e, bkO=bkO: e.activation(out=osq.t[:], in_=bkO.t[:], func=AF.Square), reads=[bkO], writes=[osq])
                bkN = nb()
                op("pe", lambda e, bkN=bkN: e.matmul(bkN.t[:], lhsT=ones_bf.t[:], rhs=osq.t[:], start=True, stop=True), reads=[ones_bf, osq], appends=[bkN])
                op("act", lambda e, bkN=bkN: e.activation(out=rn.t[:], in_=bkN.t[:], func=AF.Sqrt, bias=EPS, scale=1.0 / 128), reads=[bkN], writes=[rn])
                op("dve", lambda e: e.reciprocal(out=rn.t[:], in_=rn.t[:]), reads=[rn], writes=[rn])
                op("dve", lambda e, bkO=bkO: e.tensor_tensor(out=tmp.t[:], in0=bkO.t[:], in1=rn.t[:], op=ALU.mult), reads=[bkO, rn], writes=[tmp])
                op("dve", lambda e: e.scalar_tensor_tensor(out=yhT.t[:], in0=tmp.t[:], scalar=gcols.t[:, 24:25], in1=sgate.t[:], op0=ALU.mult, op1=ALU.mult),
                   reads=[tmp, gcols, sgate], writes=[yhT])

                wap = [next_piece(), next_piece()]
                whp = next_piece()
                whp3 = whp.t[:].rearrange("p (c n) -> p c n", c=4)
                for half in range(2):
                    wa3 = wap[half].t[:].rearrange("p (c n) -> p c n", c=8)
                    bA = nb()
                    bB = nb()
                    for oc in range(4):
                        for hh in range(8):
                            op("pe", lambda e, bA=bA, wa3=wa3, oc=oc, hh=hh: e.matmul(bA.t[:, oc * 128:(oc + 1) * 128], lhsT=wa3[0:64, hh, oc * 128:(oc + 1) * 128],
                                                                                       rhs=yT.t[0:64, hh * 128:(hh + 1) * 128], start=(hh == 0), stop=(hh == 7)),
                               reads=[wap[half], yT], appends=[bA])
                    for oc in range(4):
                        chunk = half * 4 + oc
                        for kc in range(4):
                            op("pe", lambda e, bB=bB, oc=oc, kc=kc, chunk=chunk, whp3=whp3: e.matmul(bB.t[:, oc * 128:(oc + 1) * 128], lhsT=whp3[:, kc, chunk * 128:(chunk + 1) * 128],
                                                                                           rhs=yhT.t[:, kc * 128:(kc + 1) * 128], start=(kc == 0), stop=(kc == 3)),
                               reads=[whp, yhT], appends=[bB])
                    op("dve", lambda e, bA=bA, half=half: e.tensor_tensor(out=t1.t[:], in0=bA.t[:], in1=sga.t[:, half * 512:(half + 1) * 512], op=ALU.mult),
                       reads=[bA, sga], writes=[t1])
                    op("dve", lambda e, bB=bB, half=half: e.tensor_tensor(out=t2.t[:], in0=bB.t[:], in1=sgh.t[:, half * 512:(half + 1) * 512], op=ALU.mult),
                       reads=[bB, sgh], writes=[t2])
                    op("pool", lambda e, half=half: e.tensor_tensor(out=mT.t[:, half * 512:(half + 1) * 512], in0=t1.t[:], in1=t2.t[:], op=ALU.add),
                       reads=[t1, t2], appends=[mT])
                prefetch()
                for nbk in range(2):
                    wo = next_piece()
                    wo3 = wo.t[:].rearrange("p (c n) -> p c n", c=8)
                    bX = nb()
                    for kc in range(8):
                        op("pe", lambda e, bX=bX, wo3=wo3, kc=kc: e.matmul(bX.t[:], lhsT=mT.t[:, kc * 128:(kc + 1) * 128], rhs=wo3[:, kc, :],
                                                                          start=(kc == 0), stop=(kc == 7)), reads=[wo, mT], appends=[bX])
                    prefetch()
                    op("dve", lambda e, bX=bX, nbk=nbk, Xc=Xc: e.tensor_tensor(out=X2.t[:, nbk * 512:(nbk + 1) * 512], in0=bX.t[:],
                                                                               in1=Xc.t[:, nbk * 512:(nbk + 1) * 512], op=ALU.add),
                       reads=[bX, Xc], appends=[X2])

                if STAGE == 3:
                    dump(X2, n); n += 1; continue
                r2 = rmsnorm_stats(X2, 3, None)
                op("dve", lambda e, r2=r2: e.tensor_scalar(out=H2b.t[:], in0=X2.t[:], scalar1=r2, scalar2=None, op0=ALU.mult), reads=[X2, ss], writes=[H2b])
                op("dve", lambda e, r2=r2: e.scalar_tensor_tensor(out=H2g.t[:], in0=X2.t[:], scalar=r2, in1=gffn_bc.t[:], op0=ALU.mult, op1=ALU.mult),
                   reads=[X2, ss, gffn_bc], writes=[H2g])
                transpose8(H2b, h2T)
                for pq in range(4):
                    wq = next_piece()
                    wq3 = wq.t[:].rearrange("p (c n) -> p c n", c=8)
                    bQ = nb()
                    for m in range(4):
                        for c in range(8):
                            op("pe", lambda e, bQ=bQ, wq3=wq3, m=m, c=c: e.matmul(bQ.t[:, m * 128:(m + 1) * 128], lhsT=wq3[:, c, m * 128:(m + 1) * 128],
                                                                                   rhs=h2T.t[:, c * 128:(c + 1) * 128], start=(c == 0), stop=(c == 7)),
                               reads=[wq, h2T], appends=[bQ])
                    prefetch()
                    op("act", lambda e, bQ=bQ, pq=pq: e.activation(out=qTp.t[:, pq * 512:(pq + 1) * 512], in_=bQ.t[:], func=AF.Copy), reads=[bQ], appends=[qTp])
                for pq in range(4):
                    bS = nb()
                    for m in range(4):
                        hp = pq * 4 + m
                        op("pe", lambda e, bS=bS, m=m, hp=hp: e.matmul(bS.t[:, m * 128:(m + 1) * 128], lhsT=qTp.t[:, hp * 128:(hp + 1) * 128],
                                                                       rhs=keysT.t[:, hp * 128:(hp + 1) * 128], start=True, stop=True),
                           reads=[qTp, keysT], appends=[bS])
                    op("act", lambda e, bS=bS, pq=pq: e.activation(out=SC.t[:, pq * 512:(pq + 1) * 512], in_=bS.t[:], func=AF.Copy), reads=[bS], appends=[SC])
                for hp in range(16):
                    sc_ = SC.t[:, hp * 128:(hp + 1) * 128]
                    va = V16.t[:, hp * 16:hp * 16 + 8]
                    vb = V16.t[:, hp * 16 + 8:hp * 16 + 16]
                    op("dve", lambda e, sc_=sc_, va=va: e.max(out=va, in_=sc_), reads=[SC], appends=[V16])
                    op("dve", lambda e, sc_=sc_, va=va: e.match_replace(out=SCR.t[:, 0:128], in_to_replace=va, in_values=sc_, imm_value=-1e30),
                       reads=[SC, V16], writes=[SCR])
                    op("dve", lambda e, vb=vb: e.max(out=vb, in_=SCR.t[:, 0:128]), reads=[SCR], appends=[V16])
                    op("dve", lambda e, sc_=sc_, va=va, hp=hp: e.max_index(out=I16.t[:, hp * 16:hp * 16 + 8], in_max=va, in_values=sc_),
                       reads=[SC, V16], appends=[I16])
                    op("dve", lambda e, sc_=sc_, vb=vb, hp=hp: e.max_index(out=I16.t[:, hp * 16 + 8:hp * 16 + 16], in_max=vb, in_values=sc_),
                       reads=[SC, V16], appends=[I16])
                op("dve", lambda e: e.tensor_copy(out=I16f.t[:], in_=I16.t[:]), reads=[I16], writes=[I16f])
                cand4 = BIG1.t[:].rearrange("p (h a b) -> p h a b", h=8, a=16)
                v3 = V16.t[:].rearrange("p (h q k) -> p h q k", h=8, q=2)
                for h in range(8):
                    op("dve", lambda e, h=h: e.tensor_tensor(out=cand4[:, h], in0=v3[:, h, 0, :].unsqueeze(2).to_broadcast([128, 16, 16]),
                                                             in1=v3[:, h, 1, :].unsqueeze(1).to_broadcast([128, 16, 16]), op=ALU.add),
                       reads=[V16], appends=[BIG1])
                for h in range(8):
                    cd = BIG1.t[:, h * 256:(h + 1) * 256]
                    ba = B16.t[:, h * 16:h * 16 + 8]
                    bb = B16.t[:, h * 16 + 8:h * 16 + 16]
                    op("dve", lambda e, cd=cd, ba=ba: e.max(out=ba, in_=cd), reads=[BIG1], appends=[B16])
                    op("dve", lambda e, cd=cd, ba=ba: e.match_replace(out=SCR.t[:], in_to_replace=ba, in_values=cd, imm_value=-1e30),
                       reads=[BIG1, B16], writes=[SCR])
                    op("dve", lambda e, bb=bb: e.max(out=bb, in_=SCR.t[:]), reads=[SCR], appends=[B16])
                    op("dve", lambda e, cd=cd, ba=ba, h=h: e.max_index(out=P16.t[:, h * 16:h * 16 + 8], in_max=ba, in_values=cd), reads=[BIG1, B16], appends=[P16])
                    op("dve", lambda e, cd=cd, bb=bb, h=h: e.max_index(out=P16.t[:, h * 16 + 8:h * 16 + 16], in_max=bb, in_values=cd), reads=[BIG1, B16], appends=[P16])
                b3 = B16.t[:].rearrange("p (h k) -> p h k", h=8)
                op("dve", lambda e: e.tensor_scalar(out=negm.t[:], in0=b3[:, :, 0], scalar1=-1.0, scalar2=None, op0=ALU.mult), reads=[B16], writes=[negm])
                for h in range(8):
                    op("act", lambda e, h=h: e.activation(out=E16.t[:, h * 16:(h + 1) * 16], in_=B16.t[:, h * 16:(h + 1) * 16], func=AF.Exp,
                                                          bias=negm.t[:, h:h + 1], scale=1.0, accum_out=Z.t[:, h:h + 1]),
                       reads=[B16, negm], appends=[E16, Z])
                op("dve", lambda e: e.reciprocal(out=rZ.t[:], in_=Z.t[:]), reads=[Z], writes=[rZ])
                op("dve", lambda e: e.tensor_tensor(out=E16.t[:].rearrange("p (h k) -> p h k", h=8), in0=E16.t[:].rearrange("p (h k) -> p h k", h=8),
                                                    in1=rZ.t[:].unsqueeze(2).to_broadcast([128, 8, 16]), op=ALU.mult),
                   reads=[E16, rZ], writes=[E16])
                op("dve", lambda e: e.tensor_copy(out=posf.t[:], in_=P16.t[:]), reads=[P16], writes=[posf])
                big2_3 = BIG2.t[:].rearrange("p (a b) -> p a b", b=16)
                big1_3 = BIG1.t[:].rearrange("p (a b) -> p a b", b=16)
                op("dve", lambda e: e.tensor_tensor(out=big2_3, in0=posf.t[:].unsqueeze(2).to_broadcast([128, 128, 16]),
                                                    in1=thrA.unsqueeze(1).to_broadcast([128, 128, 16]), op=ALU.is_ge),
                   reads=[posf, cst], writes=[BIG2])
                op("dve", lambda e: e.tensor_reduce(out=k1f.t[:], in_=big2_3, axis=AX.X, op=ALU.add), reads=[BIG2], writes=[k1f])
                op("dve", lambda e: e.scalar_tensor_tensor(out=k2f.t[:], in0=k1f.t[:], scalar=-16.0, in1=posf.t[:], op0=ALU.mult, op1=ALU.add),
                   reads=[k1f, posf], writes=[k2f])
                i3 = I16f.t[:].rearrange("p (h q k) -> p h q k", h=8, q=2)
                for half_, (kf, isel) in enumerate(((k1f, i1s), (k2f, i2s))):
                    op("dve", lambda e, kf=kf: e.tensor_tensor(out=big1_3, in0=iotaA.unsqueeze(1).to_broadcast([128, 128, 16]),
                                                              in1=kf.t[:].unsqueeze(2).to_broadcast([128, 128, 16]), op=ALU.is_equal),
                       reads=[kf, cst], writes=[BIG1])
                    for h in range(8):
                        op("dve", lambda e, h=h, half_=half_: e.tensor_tensor(out=big2_3[:, h * 16:(h + 1) * 16, :], in0=big1_3[:, h * 16:(h + 1) * 16, :],
                                                                               in1=i3[:, h, half_, :].unsqueeze(1).to_broadcast([128, 16, 16]), op=ALU.mult),
                           reads=[BIG1, I16f], appends=[BIG2])
                    op("dve", lambda e, isel=isel: e.tensor_reduce(out=isel.t[:], in_=big2_3, axis=AX.X, op=ALU.add), reads=[BIG2], writes=[isel])
                op("dve", lambda e: e.scalar_tensor_tensor(out=idxf.t[:], in0=i1s.t[:], scalar=128.0, in1=i2s.t[:], op0=ALU.mult, op1=ALU.add),
                   reads=[i1s, i2s], writes=[idxf])
                op("dve", lambda e: e.tensor_copy(out=IDX.t[:], in_=idxf.t[:]), reads=[idxf], writes=[IDX])
                if STAGE == 4:
                    op('dve', lambda e: e.tensor_copy(out=ACC.t[:, 0:128], in_=idxf.t[:]), reads=[idxf], writes=[ACC])
                    op('dve', lambda e: e.tensor_copy(out=ACC.t[:, 128:256], in_=E16.t[:]), reads=[E16], appends=[ACC])
                    op('dve', lambda e: e.tensor_copy(out=ACC.t[:, 256:384], in_=B16.t[:]), reads=[B16], appends=[ACC])
                    op('dve', lambda e: e.tensor_copy(out=ACC.t[:, 384:512], in_=H2g.t[:, 0:128]), reads=[H2g], appends=[ACC])
                    op('dve', lambda e: e.tensor_copy(out=ACC.t[:, 512:768], in_=V16.t[:]), reads=[V16], appends=[ACC])
                    op('dve', lambda e: e.tensor_copy(out=ACC.t[:, 768:1024], in_=qTp.t[:, 0:256]), reads=[qTp], appends=[ACC])
                    dump(ACC, n); n += 1; continue
                bacc = [banks[5], banks[6]]
                GG = 2
                NG = 128 // GG

                def emit_dots(g):
                    for kk in range(GG):
                        k = g * GG + kk
                        sl = GSb[k % 8]
                        dma("pool", lambda e, sl=sl, k=k: e.indirect_dma_start(out=sl.t[:], out_offset=None, in_=uvb_d,
                                                                               in_offset=bass.IndirectOffsetOnAxis(ap=IDX.t[:, k:k + 1], axis=0), bounds_check=breg(e), oob_is_err=False),
                            sl, reads=[IDX, uvbuf])
                        jb = junkD if k % 2 == 0 else junk
                        op("dve", lambda e, sl=sl, k=k, jb=jb: e.scalar_tensor_tensor(out=jb.t[:], in0=sl.t[:, 0:D], scalar=1.0, in1=H2g.t[:], op0=ALU.mult, op1=ALU.mult,
                                                                                      accum_out=ACTV.t[:, k:k + 1]),
                           reads=[sl, H2g], writes=[jb], appends=[ACTV])
                    op("act", lambda e, g=g: e.activation(out=GL.t[:, g * GG:(g + 1) * GG], in_=ACTV.t[:, g * GG:(g + 1) * GG], func=AF.Gelu), reads=[ACTV], appends=[GL])

                def emit_out(g):
                    for kk in range(GG):
                        k = g * GG + kk
                        sl = GSb[k % 8]
                        dg = DG[k % 4]
                        op("dve", lambda e, dg=dg, k=k: e.tensor_scalar(out=dg.t[:], in0=ident.t[:], scalar1=GL.t[:, k:k + 1], scalar2=E16.t[:, k:k + 1],
                                                                        op0=ALU.mult, op1=ALU.mult),
                           reads=[ident, GL, E16], writes=[dg])
                        for nbk in range(2):
                            op("pe", lambda e, dg=dg, sl=sl, nbk=nbk, k=k: e.matmul(bacc[nbk].t[:], lhsT=dg.t[:], rhs=sl.t[:, D + nbk * 512:D + (nbk + 1) * 512],
                                                                                     start=(k == 0), stop=(k == 127)),
                               reads=[dg, sl], appends=[bacc[nbk]])

                for g in range(NG):
                    emit_dots(g)
                    if g >= 1:
                        emit_out(g - 1)
                emit_out(NG - 1)
                for nbk in range(2):
                    op("dve", lambda e, nbk=nbk: e.tensor_tensor(out=ACC.t[:, nbk * 512:(nbk + 1) * 512], in0=bacc[nbk].t[:],
                                                                 in1=X2.t[:, nbk * 512:(nbk + 1) * 512], op=ALU.add),
                       reads=[bacc[nbk], X2], appends=[ACC])
                r3 = rmsnorm_stats(ACC, 6, None)
                op("dve", lambda e, r3=r3: e.scalar_tensor_tensor(out=X2.t[:], in0=ACC.t[:], scalar=r3, in1=gfin_bc.t[:], op0=ALU.mult, op1=ALU.mult),
                   reads=[ACC, ss, gfin_bc], writes=[X2])
                dma("pool", lambda e, n=n: e.dma_start(out=out_d[n * 128:(n + 1) * 128, :], in_=X2.t[:]), outb, reads=[X2], append=True)
                n += 1
        S.wait_all("pool", [outb])
        S.emit()
    return nc


def _host_consts():
    f = np.float32
    ident = np.eye(128, dtype=f)
    s = np.arange(128)[:, None]
    t = np.arange(128)[None, :]
    hmask = ((s <= t) & ((s // 64) == (t // 64))).astype(f)
    reset = np.ones((128, 512), f)
    reset[:, ::64] = 0.0
    iota = np.tile(np.arange(16, dtype=f)[None, :], (128, 1))
    thr = np.tile(np.array([16 * (i + 1) for i in range(15)] + [1e9], dtype=f)[None, :], (128, 1))
    cst = np.zeros((128, 1024), f)
    cst[:, 0:128] = ident
    cst[:, 128:256] = hmask
    cst[:, 256:768] = reset
    cst[:, 768:784] = iota
    cst[:, 784:800] = thr
    k = np.arange(128, dtype=np.float64)[:, None]
    q = np.arange(128, dtype=np.float64)[None, :]
    tab = np.zeros((128, 4, 4, 128), np.float64)
    for b in range(4):
        for hl in range(2):
            hh = b if hl == 0 else 4 + b
            slope = 2.0 ** (-(hh + 1))
            tab[:, b, 0 * 2 + hl, :] = np.where(k > q, np.exp(-slope * (128 + q - k)), 0.0)
            tab[:, b, 1 * 2 + hl, :] = np.where(k <= q, np.exp(-slope * (q - k)), 0.0)
    return cst, tab.reshape(128, 2048).astype(f)


def _prep_shared(norm_mix_g, w_in, attn_sinks, hgrn_lb_logits, hgrn_norm_g, w_attn_proj, w_hgrn_proj, w_out,
                 norm_ffn_g, w_peer_q, peer_keys, peer_u, peer_v, norm_final_g):
    f = np.float32
    w_in = np.asarray(w_in[0], f)
    perm = []
    for m in range(4):
        perm += list(range(m * 64, (m + 1) * 64)) + list(range((4 + m) * 64, (5 + m) * 64))
    cols = perm + list(range(512, 4864))
    w_in_p = w_in[:, cols]
    wr = w_in_p.reshape(8, 128, 4864).transpose(1, 0, 2)
    wall = np.zeros((NPIECE, 128, 4096), f)
    bounds = [(0, 512), (512, 768), (768, 1280), (1280, 1792), (1792, 2304), (2304, 2816),
              (2816, 3328), (3328, 3840), (3840, 4352), (4352, 4864)]
    for g, (a, b) in enumerate(bounds):
        blk = np.zeros((128, 8, 512), f)
        blk[:, :, :b - a] = wr[:, :, a:b]
        wall[g] = blk.reshape(128, 4096)
    wap = np.asarray(w_attn_proj[0], f).reshape(8, 64, 1024).transpose(1, 0, 2)
    for half in range(2):
        blk = np.zeros((128, 8, 512), f)
        blk[0:64] = wap[:, :, half * 512:(half + 1) * 512]
        wall[10 + half] = blk.reshape(128, 4096)
    whp = np.asarray(w_hgrn_proj[0], f).reshape(4, 128, 1024).transpose(1, 0, 2)
    wall[12] = whp.reshape(128, 4096)
    wo = np.asarray(w_out[0], f).reshape(8, 128, 1024).transpose(1, 0, 2)
    for half in range(2):
        wall[13 + half] = np.ascontiguousarray(wo[:, :, half * 512:(half + 1) * 512]).reshape(128, 4096)
    wq = np.asarray(w_peer_q[0], f).reshape(8, 128, 2048).transpose(1, 0, 2)
    for pq in range(4):
        wall[15 + pq] = np.ascontiguousarray(wq[:, :, pq * 512:(pq + 1) * 512]).reshape(128, 4096)
    keys = np.asarray(peer_keys[0], f)
    keysT = keys.transpose(3, 1, 0, 2).reshape(128, 2048)
    gcols = np.zeros((128, 32), f)
    gcols[:, 0:8] = np.asarray(norm_mix_g[0], f).reshape(8, 128).T
    gcols[:, 8:16] = np.asarray(norm_ffn_g[0], f).reshape(8, 128).T
    gcols[:, 16:20] = np.asarray(hgrn_lb_logits[0], f).reshape(4, 128).T
    gcols[:, 20:24] = np.asarray(hgrn_lb_logits[1], f).reshape(4, 128).T
    gcols[:, 24] = np.asarray(hgrn_norm_g[0], f)
    cst, tab = _host_consts()
    return {
        "wall": wall,
        "pu": np.ascontiguousarray(np.asarray(peer_u[0], f)),
        "pv": np.ascontiguousarray(np.asarray(peer_v[0], f)),
        "keysT": np.ascontiguousarray(keysT),
        "gcols": gcols,
        "sinks": np.asarray(attn_sinks, f).reshape(1, 8),
        "gffn_bc": np.ascontiguousarray(np.tile(np.asarray(norm_ffn_g[0], f)[None, :], (128, 1))),
        "gfin_bc": np.ascontiguousarray(np.tile(np.asarray(norm_final_g, f)[None, :], (128, 1))),
        "attn_tab": tab,
        "consts": cst,
    }


def run(x, params, nseq, nt):
    shared = _prep_shared(**params)
    nc = build_nc(nseq, nt)
    xs = np.ascontiguousarray(np.asarray(x, np.float32)).reshape(NCORES, nseq * nt * 128, D)
    in_maps = [dict(shared, x=xs[i]) for i in range(NCORES)]
    res = run_bass_kernel_spmd(nc, in_maps, core_ids=list(range(NCORES)))
    out = np.stack([r["out"] for r in res.results], axis=0)
    return out.reshape(NCORES * nseq, nt * 128, D).astype(np.float32)


def kernel(x, norm_mix_g, w_in, attn_sinks, hgrn_lb_logits, hgrn_norm_g, w_attn_proj, w_hgrn_proj, w_out,
           norm_ffn_g, w_peer_q, peer_keys, peer_u, peer_v, norm_final_g):
    params = dict(norm_mix_g=norm_mix_g, w_in=w_in, attn_sinks=attn_sinks, hgrn_lb_logits=hgrn_lb_logits,
                  hgrn_norm_g=hgrn_norm_g, w_attn_proj=w_attn_proj, w_hgrn_proj=w_hgrn_proj, w_out=w_out,
                  norm_ffn_g=norm_ffn_g, w_peer_q=w_peer_q, peer_keys=peer_keys, peer_u=peer_u, peer_v=peer_v,
                  norm_final_g=norm_final_g)
    x = np.asarray(x)
    B = x.shape[0]
    return run(x, params, B // NCORES, x.shape[1] // 128)
```

```python
import numpy as np
from contextlib import ExitStack
import concourse.bass as bass
import concourse.mybir as mybir
from concourse.bass_utils import run_bass_kernel_spmd

F32 = mybir.dt.float32
BF16 = mybir.dt.bfloat16
U32 = mybir.dt.uint32
AF = mybir.ActivationFunctionType
ALU = mybir.AluOpType
AX = mybir.AxisListType

D = 1024
SEQ = 4096
NCORES = 8
EPS = 1e-6
NPIECE = 19
import os
STAGE = float(os.environ.get('KSTAGE', '9'))


class Buf:
    __slots__ = ("name", "t", "writers", "readers", "dsem", "dcount", "psum")

    def __init__(self, name, t=None):
        self.psum = False
        self.name = name
        self.t = t
        self.writers = {}
        self.readers = {}
        self.dsem = None
        self.dcount = 0


class Sched:
    ENGS = ("pe", "act", "dve", "pool", "sp")

    def __init__(self, nc, es):
        self.nc = nc
        self.es = es
        self.ops = {e: [] for e in self.ENGS}
        self.seq = {e: 0 for e in self.ENGS}
        self.seen = {e: {} for e in self.ENGS}
        self.esem = {e: es.enter_context(nc.semaphore("s_" + e)) for e in self.ENGS}

    def sb(self, name, shape, dt):
        return Buf(name, self.es.enter_context(self.nc.sbuf_tensor("sb_" + name, shape, dt)))

    def ps(self, name, shape, dt):
        b = Buf(name, self.es.enter_context(self.nc.psum_tensor("ps_" + name, shape, dt)))
        b.psum = True
        return b

    def _deps(self, eng, reads, writes, appends):
        deps = {}

        def add(d):
            for k, v in d.items():
                if deps.get(k, 0) < v:
                    deps[k] = v

        for b in reads:
            add(b.writers)
            if b.psum:
                add(b.readers)
        for b in writes:
            add(b.writers)
            add(b.readers)
        for b in appends:
            add(b.readers)
        out = []
        seen = self.seen[eng]
        for k, v in deps.items():
            if k == eng and eng in ("pe", "sp"):
                continue
            if seen.get(k, 0) >= v:
                continue
            seen[k] = v
            out.append((k, v))
        return out

    def _sem(self, k):
        return self.esem[k] if isinstance(k, str) else k

    def op(self, eng, fn, reads=(), writes=(), appends=()):
        waits = self._deps(eng, reads, writes, appends)
        self.seq[eng] += 1
        n = self.seq[eng]
        for b in reads:
            if b.readers.get(eng, 0) < n:
                b.readers[eng] = n
        for b in writes:
            b.writers = {eng: n}
            b.readers = {}
        for b in appends:
            b.writers[eng] = n
        self.ops[eng].append((fn, waits, (eng, 1)))

    def dma(self, q, fn, dst, reads=(), append=False):
        if dst.dsem is None:
            dst.dsem = self.es.enter_context(self.nc.semaphore("d_" + dst.name))
        waits = self._deps(q, reads, () if append else (dst,), (dst,) if append else ())
        dst.dcount += 16
        for b in reads:
            b.readers[dst.dsem] = dst.dcount
        if append:
            dst.writers[dst.dsem] = dst.dcount
        else:
            dst.writers = {dst.dsem: dst.dcount}
            dst.readers = {}
        self.ops[q].append((fn, waits, (dst.dsem, 16)))

    def wait_all(self, eng, bufs):
        waits = self._deps(eng, bufs, (), ())
        self.ops[eng].append((None, waits, None))

    def emit(self):
        nc = self.nc
        with nc.Block() as block:
            def run(engname):
                def body(e):
                    for fn, waits, inc in self.ops[engname]:
                        for k, v in waits:
                            e.wait_ge(self._sem(k), v)
                        if fn is not None:
                            ins = fn(e)
                            if inc is not None:
                                ins.then_inc(self._sem(inc[0]), inc[1])
                return body
            block.tensor(run("pe"))
            block.scalar(run("act"))
            block.vector(run("dve"))
            block.gpsimd(run("pool"))
            block.sync(run("sp"))


def build_nc(nseq, nt):
    ntok = nseq * nt * 128
    nc = bass.Bass("TRN2", target_bir_lowering=False)

    def din(name, shape, dt=F32):
        return nc.dram_tensor(name, shape, dt, kind="ExternalInput").ap()

    x_d = din("x", [ntok, D])
    wall_d = din("wall", [NPIECE, 128, 4096])
    u_d = din("pu", [16384, D])
    v_d = din("pv", [16384, D])
    keysT_d = din("keysT", [128, 2048])
    gcols_d = din("gcols", [128, 32])
    sinks_d = din("sinks", [1, 8])
    gffn_d = din("gffn_bc", [128, D])
    gfin_d = din("gfin_bc", [128, D])
    tab_d = din("attn_tab", [128, 2048])
    cst_d = din("consts", [128, 1024])
    out_d = nc.dram_tensor("out", [ntok, D], F32, kind="ExternalOutput").ap()
    ws_d = nc.dram_tensor("ws_bf", [NPIECE, 128, 4096], BF16, kind="Internal").ap()
    uvb_d = nc.dram_tensor("uvb_bf", [16384, 2 * D], BF16, kind="Internal").ap()

    with ExitStack() as es:
        S = Sched(nc, es)
        sb, op, dma = S.sb, S.op, S.dma

        ring = [sb(f"ring{i}", [128, 4096], BF16) for i in range(4)]
        X = [sb(f"X{i}", [128, D], F32) for i in range(2)]
        GSb = [sb(f"GSb{i}", [128, 2 * D], BF16) for i in range(8)]
        DG = [sb(f"DG{i}", [128, 128], BF16) for i in range(4)]
        keysT = sb("keysT", [128, 2048], BF16)
        gcols = sb("gcols", [128, 32], F32)
        lbc = sb("lbc", [128, 4], F32)
        omlb = sb("omlb", [128, 4], F32)
        sinks = sb("sinks", [1, 8], F32)
        esrow = sb("esrow", [1, 8, 64], BF16)
        onesrow = sb("onesrow", [1, 128], BF16)
        ones_bf = sb("ones_bf", [128, 128], BF16)
        ident = sb("ident", [128, 128], BF16)
        gffn_bc = sb("gffn_bc", [128, D], F32)
        gfin_bc = sb("gfin_bc", [128, D], F32)
        tab = sb("tab", [128, 2048], F32)
        cst = sb("cst", [128, 1024], F32)
        ws = Buf("ws")
        parts = [Buf(f"wpart{i}") for i in range(8)]
        outb = Buf("outb")

        Hb = sb("Hb", [128, D], BF16)
        H2b = sb("H2b", [128, D], BF16)
        hT = sb("hT", [128, D], BF16)
        h2T = sb("h2T", [128, D], BF16)
        ss = sb("ss", [128, 12], F32)
        qT = sb("qT", [128, 1024], BF16)
        KT = [sb(f"KT{i}", [128, 128], BF16) for i in range(2)]
        V = [sb(f"V{i}", [128, 128], BF16) for i in range(2)]
        qs = sb("qs", [128, 512], F32)
        fT = sb("fT", [128, 512], F32)
        logf = sb("logf", [128, 512], F32)
        kT = sb("kT", [128, 512], F32)
        bT = sb("bT", [128, 512], F32)
        eb = sb("eb", [128, 512], F32)
        enb = sb("enb", [128, 512], F32)
        qd = sb("qd", [128, 512], BF16)
        kinv = sb("kinv", [128, 512], BF16)
        kdecT = sb("kdecT", [128, 512], BF16)
        kdec = sb("kdec", [128, 512], BF16)
        VH = sb("VH", [128, 512], BF16)
        sgate = sb("sgate", [128, 512], BF16)
        sga = sb("sga", [128, D], BF16)
        sgh = sb("sgh", [128, D], BF16)
        ATm = sb("ATm", [128, 512], BF16)
        osq = sb("osq", [128, 512], BF16)
        rn = sb("rn", [128, 512], F32)
        tmp = sb("tmp", [128, 512], F32)
        yhT = sb("yhT", [128, 512], BF16)
        yT = sb("yT", [64, D], BF16)
        rz = sb("rz", [64, D], F32)
        pe32 = [sb(f"pe32_{i}", [128, 512], F32) for i in range(2)]
        PT = [sb(f"PT{i}", [128, 512], BF16) for i in range(4)]
        mT = sb("mT", [128, D], BF16)
        t1 = pe32[0]
        t2 = pe32[1]
        S32 = [sb(f"S32_{i}", [128, 128], F32) for i in range(4)]
        Sbf = [sb(f"Sbf_{i}", [128, 128], BF16) for i in range(4)]
        X2 = sb("X2", [128, D], F32)
        ACC = sb("ACC", [128, D], F32)
        H2g = sb("H2g", [128, D], F32)
        junk = sb("junk", [128, D], BF16)
        junkD = sb("junkD", [128, D], BF16)
        qTp = sb("qTp", [128, 2048], BF16)
        SC = sb("SC", [128, 2048], F32)
        SCR = sb("SCR", [128, 256], F32)
        V16 = sb("V16", [128, 256], F32)
        I16 = sb("I16", [128, 256], U32)
        I16f = sb("I16f", [128, 256], F32)
        BIG1 = sb("BIG1", [128, 2048], F32)
        BIG2 = SC
        B16 = sb("B16", [128, 128], F32)
        P16 = sb("P16", [128, 128], U32)
        posf = sb("posf", [128, 128], F32)
        k1f = sb("k1f", [128, 128], F32)
        k2f = sb("k2f", [128, 128], F32)
        i1s = sb("i1s", [128, 128], F32)
        i2s = sb("i2s", [128, 128], F32)
        idxf = sb("idxf", [128, 128], F32)
        IDX = sb("IDX", [128, 128], U32)
        E16 = sb("E16", [128, 128], F32)
        negm = sb("negm", [128, 8], F32)
        Z = sb("Z", [128, 8], F32)
        rZ = sb("rZ", [128, 8], F32)
        ACTV = sb("ACTV", [128, 128], F32)
        GL = sb("GL", [128, 128], F32)
        Wt = sb("Wt", [128, 128], F32)

        banks = [S.ps(f"bank{i}", [128, 512], F32) for i in range(8)]
        bstate = [0]

        def nb():
            b = banks[bstate[0] % 5]
            bstate[0] += 1
            return b

        identA = cst.t[:, 0:128]
        hmaskA = cst.t[:, 128:256]
        resetA = cst.t[:, 256:768]
        iotaA = cst.t[:, 768:784]
        thrA = cst.t[:, 784:800]

        dma("sp", lambda e: e.dma_start(out=gcols.t[:], in_=gcols_d), gcols)
        dma("sp", lambda e: e.dma_start(out=sinks.t[:], in_=sinks_d), sinks)
        dma("sp", lambda e: e.dma_start(out=gffn_bc.t[:], in_=gffn_d), gffn_bc)
        dma("sp", lambda e: e.dma_start(out=gfin_bc.t[:], in_=gfin_d), gfin_bc)
        dma("sp", lambda e: e.dma_start(out=tab.t[:], in_=tab_d), tab)
        dma("sp", lambda e: e.dma_start(out=cst.t[:], in_=cst_d), cst)
        op("dve", lambda e: e.tensor_copy(out=ident.t[:], in_=identA), reads=[cst], writes=[ident])
        op("pool", lambda e: e.memset(ones_bf.t[:], 1.0), writes=[ones_bf])
        op("pool", lambda e: e.memset(onesrow.t[:], 1.0), writes=[onesrow])
        op("pool", lambda e: e.memset(qT.t[:], 0.0), writes=[qT])
        op("dve", lambda e: e.tensor_tensor(out=lbc.t[:], in0=gcols.t[:, 16:20], in1=gcols.t[:, 20:24], op=ALU.subtract),
           reads=[gcols], writes=[lbc])
        op("act", lambda e: e.activation(out=lbc.t[:], in_=lbc.t[:], func=AF.Sigmoid), reads=[lbc], writes=[lbc])
        op("dve", lambda e: e.tensor_scalar(out=omlb.t[:], in0=lbc.t[:], scalar1=-1.0, scalar2=1.0, op0=ALU.mult, op1=ALU.add),
           reads=[lbc], writes=[omlb])
        op("act", lambda e: e.activation(out=sinks.t[:], in_=sinks.t[:], func=AF.Exp), reads=[sinks], writes=[sinks])
        op("dve", lambda e: e.tensor_copy(out=esrow.t[:], in_=sinks.t[:].unsqueeze(2).to_broadcast([1, 8, 64])),
           reads=[sinks], writes=[esrow])
        st32 = [X[0], X[1], X2, ACC]
        for q in range(2):
            st = st32[q]
            dma("sp", lambda e, st=st, q=q: e.dma_start(out=st.t[:], in_=keysT_d[:, q * 1024:(q + 1) * 1024]), st)
            op("dve", lambda e, st=st, q=q: e.tensor_copy(out=keysT.t[:, q * 1024:(q + 1) * 1024], in_=st.t[:]),
               reads=[st], appends=[keysT])

        pro = []
        it = 0
        for tbl_d, coff in ((u_d, 0), (v_d, D)):
            for r in range(128):
                def mk(it=it, tbl_d=tbl_d, coff=coff, r=r):
                    st = st32[it % 4]
                    sv = GSb[it % 8]

                    def load():
                        dma("sp", lambda e: e.dma_start(out=st.t[:], in_=tbl_d[r * 128:(r + 1) * 128, :]), st)

                    def work():
                        eng = ("dve", "pool", "act")[it % 3]
                        if eng == "act":
                            op("act", lambda e: e.activation(out=sv.t[:, 0:D], in_=st.t[:], func=AF.Copy), reads=[st], writes=[sv])
                        else:
                            op(eng, lambda e: e.tensor_copy(out=sv.t[:, 0:D], in_=st.t[:]), reads=[st], writes=[sv])
                        dma("sp", lambda e: e.dma_start(out=uvb_d[r * 128:(r + 1) * 128, coff:coff + D], in_=sv.t[:, 0:D]),
                            parts[it % 8], reads=[sv], append=True)
                    return load, work
                pro.append(mk())
                it += 1
        for p in range(NPIECE):
            for q in range(4):
                def mk(it=it, p=p, q=q):
                    st = st32[it % 4]
                    sv = GSb[it % 8]
                    svb = sv.t[:]

                    def load():
                        dma("sp", lambda e: e.dma_start(out=st.t[:], in_=wall_d[p, :, q * 1024:(q + 1) * 1024]), st)

                    def work():
                        for hf_ in range(2):
                            c = q * 2 + hf_
                            src = st.t[:, hf_ * 512:(hf_ + 1) * 512]
                            dst = svb[:, hf_ * 512:(hf_ + 1) * 512]
                            eng = "dve" if (it + hf_) % 2 == 0 else "pool"
                            if p < 10:
                                sc = gcols.t[:, c:c + 1]
                            elif p >= 15:
                                sc = gcols.t[:, 8 + c:9 + c]
                            else:
                                sc = None
                            if sc is not None:
                                op(eng, lambda e, src=src, dst=dst, sc=sc: e.tensor_scalar(out=dst, in0=src, scalar1=sc, scalar2=None, op0=ALU.mult),
                                   reads=[st, gcols], appends=[sv])
                            else:
                                op(eng, lambda e, src=src, dst=dst: e.tensor_copy(out=dst, in_=src), reads=[st], appends=[sv])
                        dma("sp", lambda e: e.dma_start(out=ws_d[p, :, q * 1024:(q + 1) * 1024], in_=svb[:, 0:1024]),
                            parts[it % 8], reads=[sv], append=True)
                    return load, work
                pro.append(mk())
                it += 1
        LA = 3
        for i in range(min(LA, len(pro))):
            pro[i][0]()
        for i in range(len(pro)):
            if i + LA < len(pro):
                pro[i + LA][0]()
            pro[i][1]()

        total_uses = nseq * nt * NPIECE
        rstate = {"loaded": 0}

        def ring_fetch_upto(n):
            while rstate["loaded"] < min(n, total_uses):
                i = rstate["loaded"]
                slot = ring[i % 4]
                p = i % NPIECE
                dma("sp", lambda e, slot=slot, p=p: e.dma_start(out=slot.t[:], in_=ws_d[p, :, :]), slot, reads=parts)
                rstate["loaded"] += 1

        use_ctr = [0]

        def next_piece():
            i = use_ctr[0]
            ring_fetch_upto(i + 1)
            use_ctr[0] += 1
            return ring[i % 4]

        def prefetch():
            ring_fetch_upto(use_ctr[0] + 4)

        def load_x(n):
            xb = X[n % 2]
            dma("sp", lambda e, xb=xb, n=n: e.dma_start(out=xb.t[:], in_=x_d[n * 128:(n + 1) * 128, :]), xb)

        def rmsnorm_stats(src, col, name_reads):
            op("act", lambda e: e.activation(out=junk.t[:], in_=src.t[:], func=AF.Square, accum_out=ss.t[:, col:col + 1]),
               reads=[src], writes=[junk], appends=[ss])
            op("act", lambda e: e.activation(out=ss.t[:, col + 1:col + 2], in_=ss.t[:, col:col + 1], func=AF.Sqrt, bias=EPS, scale=1.0 / D),
               reads=[ss], appends=[ss])
            op("dve", lambda e: e.reciprocal(out=ss.t[:, col + 2:col + 3], in_=ss.t[:, col + 1:col + 2]), reads=[ss], appends=[ss])
            return ss.t[:, col + 2:col + 3]

        def transpose8(src, dst):
            bk = nb()
            bkb = bk.t[:].bitcast(BF16)
            for c in range(8):
                op("pe", lambda e, c=c: e.transpose(out=bkb[:, c * 128:(c + 1) * 128], in_=src.t[:, c * 128:(c + 1) * 128], identity=ident.t[:]),
                   reads=[src, ident], appends=[bk])
            op("act", lambda e: e.activation(out=dst.t[:], in_=bkb[:, 0:1024], func=AF.Copy), reads=[bk], writes=[dst])

        def fm_group(wb, ncols128, evac):
            bk = nb()
            w3 = wb.t[:].rearrange("p (c n) -> p c n", c=8)
            for m in range(ncols128):
                for c in range(8):
                    op("pe", lambda e, m=m, c=c: e.matmul(bk.t[:, m * 128:(m + 1) * 128], lhsT=w3[:, c, m * 128:(m + 1) * 128],
                                                           rhs=hT.t[:, c * 128:(c + 1) * 128], start=(c == 0), stop=(c == 7)),
                       reads=[wb, hT], appends=[bk])
            prefetch()
            evac(bk)

        def tm_group(wb, col0, ncols, bk, bcol0):
            w3 = wb.t[:].rearrange("p (c n) -> p c n", c=8)
            for c in range(8):
                op("pe", lambda e, c=c: e.matmul(bk.t[:, bcol0:bcol0 + ncols], lhsT=hT.t[:, c * 128:(c + 1) * 128],
                                                  rhs=w3[:, c, col0:col0 + ncols], start=(c == 0), stop=(c == 7)),
                   reads=[wb, hT], appends=[bk])

        def dump(src, n):
            dma("pool", lambda e, n=n, src=src: e.dma_start(out=out_d[n * 128:(n + 1) * 128, :], in_=src.t[:]), outb, reads=[src], append=True)

        regc = {}

        def breg(e):
            if "r" not in regc:
                regc["r"] = e.to_reg(16383)
            return regc["r"]

        load_x(0)
        n = 0
        for s in range(nseq):
            for hh in range(4):
                op("pool", lambda e, hh=hh: e.memset(S32[hh].t[:], 0.0), writes=[S32[hh]])
                op("pool", lambda e, hh=hh: e.memset(Sbf[hh].t[:], 0.0), writes=[Sbf[hh]])
            for j in range(nt):
                if n + 1 < nseq * nt:
                    load_x(n + 1)
                Xc = X[n % 2]
                cur, prv = j % 2, (j + 1) % 2
                if STAGE == 0:
                    dump(Xc, n); n += 1; continue
                r1 = rmsnorm_stats(Xc, 0, None)
                op("dve", lambda e, Xc=Xc, r1=r1: e.tensor_scalar(out=Hb.t[:], in0=Xc.t[:], scalar1=r1, scalar2=None, op0=ALU.mult),
                   reads=[Xc, ss], writes=[Hb])
                if STAGE == 0.3:
                    dump(Xc, n); n += 1; continue
                transpose8(Hb, hT)
                if STAGE == 0.6:
                    dump(Xc, n); n += 1; continue
                wb = next_piece()
                def evq(bk):
                    op("act", lambda e: e.activation(out=qT.t[0:64, 0:512], in_=bk.t[0:64, :], func=AF.Copy), reads=[bk], appends=[qT])
                    op("act", lambda e: e.activation(out=qT.t[64:128, 512:1024], in_=bk.t[64:128, :], func=AF.Copy), reads=[bk], appends=[qT])
                fm_group(wb, 4, evq)
                if STAGE == 0.7:
                    dump(Xc, n); n += 1; continue
                wb = next_piece()
                bk = nb()
                w3 = wb.t[:].rearrange("p (c n) -> p c n", c=8)
                for c in range(8):
                    op("pe", lambda e, c=c, w3=w3, bk=bk: e.matmul(bk.t[:, 0:128], lhsT=w3[:, c, 0:128], rhs=hT.t[:, c * 128:(c + 1) * 128],
                                                                  start=(c == 0), stop=(c == 7)), reads=[wb, hT], appends=[bk])
                tm_group(wb, 128, 128, bk, 128)
                prefetch()
                op("act", lambda e, bk=bk, cur=cur: e.activation(out=KT[cur].t[:], in_=bk.t[:, 0:128], func=AF.Copy), reads=[bk], writes=[KT[cur]])
                op("dve", lambda e, bk=bk, cur=cur: e.tensor_copy(out=V[cur].t[:], in_=bk.t[:, 128:256]), reads=[bk], writes=[V[cur]])
                if STAGE == 0.8:
                    dump(Xc, n); n += 1; continue
                wb = next_piece()
                fm_group(wb, 4, lambda bk: op("act", lambda e: e.activation(out=qs.t[:], in_=bk.t[:], func=AF.Silu), reads=[bk], writes=[qs]))
                wb = next_piece()
                fm_group(wb, 4, lambda bk: op("act", lambda e: e.activation(out=fT.t[:], in_=bk.t[:], func=AF.Sigmoid), reads=[bk], writes=[fT]))
                if STAGE == 0.9:
                    dump(Xc, n); n += 1; continue
                wb = next_piece()
                bk = nb()
                tm_group(wb, 0, 512, bk, 0)
                prefetch()
                op("dve", lambda e, bk=bk: e.tensor_copy(out=VH.t[:], in_=bk.t[:]), reads=[bk], writes=[VH])
                wb = next_piece()
                fm_group(wb, 4, lambda bk: op("act", lambda e: e.activation(out=sgate.t[:], in_=bk.t[:], func=AF.Silu), reads=[bk], writes=[sgate]))
                for half in range(2):
                    wb = next_piece()
                    fm_group(wb, 4, lambda bk, half=half: op("act", lambda e: e.activation(out=sga.t[:, half * 512:(half + 1) * 512], in_=bk.t[:], func=AF.Sigmoid),
                                                             reads=[bk], appends=[sga]))
                for half in range(2):
                    wb = next_piece()
                    fm_group(wb, 4, lambda bk, half=half: op("act", lambda e: e.activation(out=sgh.t[:, half * 512:(half + 1) * 512], in_=bk.t[:], func=AF.Sigmoid),
                                                             reads=[bk], appends=[sgh]))

                if STAGE == 1:
                    dump(Xc, n); n += 1; continue
                blks = [1] if j == 0 else [0, 1]
                lo = 256 if j == 0 else 0
                for b in range(4):
                    bk = nb()
                    for blk in blks:
                        Kb = KT[prv] if blk == 0 else KT[cur]
                        for hl in range(2):
                            col = (blk * 2 + hl) * 128
                            hq0 = (b if hl == 0 else 4 + b) * 128
                            op("pe", lambda e, bk=bk, Kb=Kb, col=col, hq0=hq0: e.matmul(
                                bk.t[:, col:col + 128], lhsT=Kb.t[:, :], rhs=qT.t[:, hq0:hq0 + 128], start=True, stop=True),
                               reads=[Kb, qT], appends=[bk])
                    pe_ = pe32[b % 2]
                    op("act", lambda e, bk=bk, pe_=pe_, lo=lo: e.activation(out=pe_.t[:, lo:512], in_=bk.t[:, lo:512], func=AF.Exp, scale=0.125),
                       reads=[bk], writes=[pe_])
                    op("dve", lambda e, pe_=pe_, b=b, lo=lo: e.tensor_tensor(out=PT[b].t[:, lo:512], in0=pe_.t[:, lo:512],
                                                                             in1=tab.t[:, b * 512 + lo:(b + 1) * 512], op=ALU.mult),
                       reads=[pe_, tab], writes=[PT[b]])
                for g in range(2):
                    po = nb()
                    pz = nb()
                    for hq_ in range(4):
                        hh = g * 4 + hq_
                        b, hl = (hh, 0) if hh < 4 else (hh - 4, 1)
                        c0 = hq_ * 128
                        for bi, blk in enumerate(blks):
                            Vb = V[prv] if blk == 0 else V[cur]
                            col = (blk * 2 + hl) * 128
                            last = (bi == len(blks) - 1)
                            op("pe", lambda e, po=po, Vb=Vb, hl=hl, b=b, col=col, c0=c0, bi=bi, last=last: e.matmul(
                                po.t[0:64, c0:c0 + 128], lhsT=Vb.t[:, hl * 64:(hl + 1) * 64], rhs=PT[b].t[:, col:col + 128],
                                start=(bi == 0), stop=last), reads=[Vb, PT[b]], appends=[po])
                        for bi, blk in enumerate(blks):
                            col = (blk * 2 + hl) * 128
                            op("pe", lambda e, pz=pz, b=b, col=col, c0=c0, bi=bi: e.matmul(
                                pz.t[0:64, c0:c0 + 128], lhsT=ones_bf.t[:, 0:64], rhs=PT[b].t[:, col:col + 128],
                                start=(bi == 0), stop=False), reads=[ones_bf, PT[b]], appends=[pz])
                        op("pe", lambda e, pz=pz, hh=hh, c0=c0: e.matmul(
                            pz.t[0:64, c0:c0 + 128], lhsT=esrow.t[0:1, hh, :], rhs=onesrow.t[0:1, :], start=False, stop=True),
                           reads=[esrow, onesrow], appends=[pz])
                    op("dve", lambda e, pz=pz, g=g: e.reciprocal(out=rz.t[:, g * 512:(g + 1) * 512], in_=pz.t[0:64, :]), reads=[pz], appends=[rz])
                    op("dve", lambda e, po=po, g=g: e.tensor_tensor(out=yT.t[:, g * 512:(g + 1) * 512], in0=po.t[0:64, :],
                                                                    in1=rz.t[:, g * 512:(g + 1) * 512], op=ALU.mult),
                       reads=[po, rz], appends=[yT])

                if STAGE == 2:
                    dump(Xc, n); n += 1; continue
                for hh in range(4):
                    op("dve", lambda e, hh=hh: e.tensor_scalar(out=fT.t[:, hh * 128:(hh + 1) * 128], in0=fT.t[:, hh * 128:(hh + 1) * 128],
                                                               scalar1=omlb.t[:, hh:hh + 1], scalar2=lbc.t[:, hh:hh + 1], op0=ALU.mult, op1=ALU.add),
                       reads=[fT, omlb, lbc], appends=[fT])
                op("act", lambda e: e.activation(out=logf.t[:], in_=fT.t[:], func=AF.Ln), reads=[fT], writes=[logf])
                op("dve", lambda e: e.tensor_scalar(out=kT.t[:], in0=fT.t[:], scalar1=-1.0, scalar2=1.0, op0=ALU.mult, op1=ALU.add),
                   reads=[fT], writes=[kT])
                op("dve", lambda e: e.tensor_tensor_scan(out=bT.t[:], data0=resetA, data1=logf.t[:], initial=0.0, op0=ALU.mult, op1=ALU.add),
                   reads=[cst, logf], writes=[bT])
                op("act", lambda e: e.activation(out=eb.t[:], in_=bT.t[:], func=AF.Exp), reads=[bT], writes=[eb])
                op("act", lambda e: e.activation(out=enb.t[:], in_=bT.t[:], func=AF.Exp, scale=-1.0), reads=[bT], writes=[enb])
                op("dve", lambda e: e.tensor_tensor(out=qd.t[:], in0=qs.t[:], in1=eb.t[:], op=ALU.mult), reads=[qs, eb], writes=[qd])
                op("dve", lambda e: e.tensor_tensor(out=kT.t[:], in0=kT.t[:], in1=enb.t[:], op=ALU.mult), reads=[kT, enb], writes=[kT])
                op("pool", lambda e: e.tensor_copy(out=kinv.t[:], in_=kT.t[:]), reads=[kT], writes=[kinv])
                for hh in range(4):
                    for ch in range(2):
                        c0 = hh * 128 + ch * 64
                        op("dve", lambda e, c0=c0: e.tensor_scalar(out=kdecT.t[:, c0:c0 + 64], in0=kT.t[:, c0:c0 + 64],
                                                                   scalar1=eb.t[:, c0 + 63:c0 + 64], scalar2=None, op0=ALU.mult),
                           reads=[kT, eb], appends=[kdecT])
                bkA = nb()
                for hh in range(4):
                    op("pe", lambda e, hh=hh, bkA=bkA: e.matmul(bkA.t[:, hh * 128:(hh + 1) * 128], lhsT=kinv.t[:, hh * 128:(hh + 1) * 128],
                                                                rhs=qd.t[:, hh * 128:(hh + 1) * 128], start=True, stop=True),
                       reads=[kinv, qd], appends=[bkA])
                op("dve", lambda e, bkA=bkA: e.tensor_tensor(out=ATm.t[:].rearrange("p (h t) -> p h t", h=4),
                                                             in0=bkA.t[:].rearrange("p (h t) -> p h t", h=4),
                                                             in1=hmaskA.unsqueeze(1).to_broadcast([128, 4, 128]), op=ALU.mult),
                   reads=[bkA, cst], writes=[ATm])
                bkK = nb()
                bkKb = bkK.t[:].bitcast(BF16)
                for hh in range(4):
                    op("pe", lambda e, hh=hh, bkKb=bkKb: e.transpose(out=bkKb[:, hh * 128:(hh + 1) * 128], in_=kdecT.t[:, hh * 128:(hh + 1) * 128],
                                                                     identity=ident.t[:]), reads=[kdecT, ident], appends=[bkK])
                op("act", lambda e, bkKb=bkKb: e.activation(out=kdec.t[:], in_=bkKb[:, 0:512], func=AF.Copy), reads=[bkK], writes=[kdec])
                bkO = banks[7]
                for hh in range(4):
                    c0 = hh * 128
                    op("pe", lambda e, c0=c0, hh=hh, bkO=bkO: e.matmul(bkO.t[:, c0:c0 + 128], lhsT=VH.t[:, c0:c0 + 128], rhs=ATm.t[:, c0:c0 + 128],
                                                                       start=True, stop=False), reads=[VH, ATm], appends=[bkO])
                    for ch in range(2):
                        cc = c0 + ch * 64
                        op("pe", lambda e, cc=cc, hh=hh, ch=ch, bkO=bkO: e.matmul(bkO.t[:, cc:cc + 64], lhsT=Sbf[hh].t[:], rhs=qd.t[:, cc:cc + 64],
                                                                                  start=False, stop=(ch == 1)), reads=[Sbf[hh], qd], appends=[bkO])
                        bkD = nb()
                        r0 = ch * 64
                        op("pe", lambda e, bkD=bkD, r0=r0, c0=c0: e.matmul(bkD.t[:, 0:128], lhsT=kdec.t[r0:r0 + 64, c0:c0 + 128],
                                                                           rhs=VH.t[r0:r0 + 64, c0:c0 + 128], start=True, stop=True),
                           reads=[kdec, VH], appends=[bkD])
                        op("dve", lambda e, bkD=bkD, hh=hh, cc=cc: e.scalar_tensor_tensor(out=S32[hh].t[:], in0=S32[hh].t[:], scalar=eb.t[:, cc + 63:cc + 64],
                                                                                          in1=bkD.t[:, 0:128], op0=ALU.mult, op1=ALU.add),
                           reads=[bkD, eb, S32[hh]], writes=[S32[hh]])
                        op("act", lambda e, hh=hh: e.activation(out=Sbf[hh].t[:], in_=S32[hh].t[:], func=AF.Copy), reads=[S32[hh]], writes=[Sbf[hh]])
                op("act", lambda e, bkO=bkO: e.activation(out=osq.t[:], in_=bkO.t[:], func=AF.Square), reads=[bkO], writes=[osq])
                bkN = nb()
                op("pe", lambda e, bkN=bkN: e.matmul(bkN.t[:], lhsT=ones_bf.t[:], rhs=osq.t[:], start=True, stop=True), reads=[ones_bf, osq], appends=[bkN])
                op("act", lambda e, bkN=bkN: e.activation(out=rn.t[:], in_=bkN.t[:], func=AF.Sqrt, bias=EPS, scale=1.0 / 128), reads=[bkN], writes=[rn])
                op("dve", lambda e: e.reciprocal(out=rn.t[:], in_=rn.t[:]), reads=[rn], writes=[rn])
                op("dve", lambda e, bkO=bkO: e.tensor_tensor(out=tmp.t[:], in0=bkO.t[:], in1=rn.t[:], op=ALU.mult), reads=[bkO, rn], writes=[tmp])
                op("dve", lambda e: e.scalar_tensor_tensor(out=yhT.t[:], in0=tmp.t[:], scalar=gcols.t[:, 24:25], in1=sgate.t[:], op0=ALU.mult, op1=ALU.mult),
                   reads=[tmp, gcols, sgate], writes=[yhT])

                wap = [next_piece(), next_piece()]
                whp = next_piece()
                whp3 = whp.t[:].rearrange("p (c n) -> p c n", c=4)
                for half in range(2):
                    wa3 = wap[half].t[:].rearrange("p (c n) -> p c n", c=8)
                    bA = nb()
                    bB = nb()
                    for oc in range(4):
                        for hh in range(8):
                            op("pe", lambda e, bA=bA, wa3=wa3, oc=oc, hh=hh: e.matmul(bA.t[:, oc * 128:(oc + 1) * 128], lhsT=wa3[0:64, hh, oc * 128:(oc + 1) * 128],
                                                                                       rhs=yT.t[0:64, hh * 128:(hh + 1) * 128], start=(hh == 0), stop=(hh == 7)),
                               reads=[wap[half], yT], appends=[bA])
                    for oc in range(4):
                        chunk = half * 4 + oc
                        for kc in range(4):
                            op("pe", lambda e, bB=bB, oc=oc, kc=kc, chunk=chunk, whp3=whp3: e.matmul(bB.t[:, oc * 128:(oc + 1) * 128], lhsT=whp3[:, kc, chunk * 128:(chunk + 1) * 128],
                                                                                           rhs=yhT.t[:, kc * 128:(kc + 1) * 128], start=(kc == 0), stop=(kc == 3)),
                               reads=[whp, yhT], appends=[bB])
                    op("dve", lambda e, bA=bA, half=half: e.tensor_tensor(out=t1.t[:], in0=bA.t[:], in1=sga.t[:, half * 512:(half + 1) * 512], op=ALU.mult),
                       reads=[bA, sga], writes=[t1])
                    op("dve", lambda e, bB=bB, half=half: e.tensor_tensor(out=t2.t[:], in0=bB.t[:], in1=sgh.t[:, half * 512:(half + 1) * 512], op=ALU.mult),
                       reads=[bB, sgh], writes=[t2])
                    op("pool", lambda e, half=half: e.tensor_tensor(out=mT.t[:, half * 512:(half + 1) * 512], in0=t1.t[:], in1=t2.t[:], op=ALU.add),
                       reads=[t1, t2], appends=[mT])
                prefetch()
                for nbk in range(2):
                    wo = next_piece()
                    wo3 = wo.t[:].rearrange("p (c n) -> p c n", c=8)
                    bX = nb()
                    for kc in range(8):
                        op("pe", lambda e, bX=bX, wo3=wo3, kc=kc: e.matmul(bX.t[:], lhsT=mT.t[:, kc * 128:(kc + 1) * 128], rhs=wo3[:, kc, :],
                                                                          start=(kc == 0), stop=(kc == 7)), reads=[wo, mT], appends=[bX])
                    prefetch()
                    op("dve", lambda e, bX=bX, nbk=nbk, Xc=Xc: e.tensor_tensor(out=X2.t[:, nbk * 512:(nbk + 1) * 512], in0=bX.t[:],
                                                                               in1=Xc.t[:, nbk * 512:(nbk + 1) * 512], op=ALU.add),
                       reads=[bX, Xc], appends=[X2])

                if STAGE == 3:
                    dump(X2, n); n += 1; continue
                r2 = rmsnorm_stats(X2, 3, None)
                op("dve", lambda e, r2=r2: e.tensor_scalar(out=H2b.t[:], in0=X2.t[:], scalar1=r2, scalar2=None, op0=ALU.mult), reads=[X2, ss], writes=[H2b])
                op("dve", lambda e, r2=r2: e.scalar_tensor_tensor(out=H2g.t[:], in0=X2.t[:], scalar=r2, in1=gffn_bc.t[:], op0=ALU.mult, op1=ALU.mult),
                   reads=[X2, ss, gffn_bc], writes=[H2g])
                transpose8(H2b, h2T)
                for pq in range(4):
                    wq = next_piece()
                    wq3 = wq.t[:].rearrange("p (c n) -> p c n", c=8)
                    bQ = nb()
                    for m in range(4):
                        for c in range(8):
                            op("pe", lambda e, bQ=bQ, wq3=wq3, m=m, c=c: e.matmul(bQ.t[:, m * 128:(m + 1) * 128], lhsT=wq3[:, c, m * 128:(m + 1) * 128],
                                                                                   rhs=h2T.t[:, c * 128:(c + 1) * 128], start=(c == 0), stop=(c == 7)),
                               reads=[wq, h2T], appends=[bQ])
                    prefetch()
                    op("act", lambda e, bQ=bQ, pq=pq: e.activation(out=qTp.t[:, pq * 512:(pq + 1) * 512], in_=bQ.t[:], func=AF.Copy), reads=[bQ], appends=[qTp])
                for pq in range(4):
                    bS = nb()
                    for m in range(4):
                        hp = pq * 4 + m
                        op("pe", lambda e, bS=bS, m=m, hp=hp: e.matmul(bS.t[:, m * 128:(m + 1) * 128], lhsT=qTp.t[:, hp * 128:(hp + 1) * 128],
                                                                       rhs=keysT.t[:, hp * 128:(hp + 1) * 128], start=True, stop=True),
                           reads=[qTp, keysT], appends=[bS])
                    op("act", lambda e, bS=bS, pq=pq: e.activation(out=SC.t[:, pq * 512:(pq + 1) * 512], in_=bS.t[:], func=AF.Copy), reads=[bS], appends=[SC])
                for hp in range(16):
                    sc_ = SC.t[:, hp * 128:(hp + 1) * 128]
                    va = V16.t[:, hp * 16:hp * 16 + 8]
                    vb = V16.t[:, hp * 16 + 8:hp * 16 + 16]
                    op("dve", lambda e, sc_=sc_, va=va: e.max(out=va, in_=sc_), reads=[SC], appends=[V16])
                    op("dve", lambda e, sc_=sc_, va=va: e.match_replace(out=SCR.t[:, 0:128], in_to_replace=va, in_values=sc_, imm_value=-1e30),
                       reads=[SC, V16], writes=[SCR])
                    op("dve", lambda e, vb=vb: e.max(out=vb, in_=SCR.t[:, 0:128]), reads=[SCR], appends=[V16])
                    op("dve", lambda e, sc_=sc_, va=va, hp=hp: e.max_index(out=I16.t[:, hp * 16:hp * 16 + 8], in_max=va, in_values=sc_),
                       reads=[SC, V16], appends=[I16])
                    op("dve", lambda e, sc_=sc_, vb=vb, hp=hp: e.max_index(out=I16.t[:, hp * 16 + 8:hp * 16 + 16], in_max=vb, in_values=sc_),
                       reads=[SC, V16], appends=[I16])
                op("dve", lambda e: e.tensor_copy(out=I16f.t[:], in_=I16.t[:]), reads=[I16], writes=[I16f])
                cand4 = BIG1.t[:].rearrange("p (h a b) -> p h a b", h=8, a=16)
                v3 = V16.t[:].rearrange("p (h q k) -> p h q k", h=8, q=2)
                for h in range(8):
                    op("dve", lambda e, h=h: e.tensor_tensor(out=cand4[:, h], in0=v3[:, h, 0, :].unsqueeze(2).to_broadcast([128, 16, 16]),
                                                             in1=v3[:, h, 1, :].unsqueeze(1).to_broadcast([128, 16, 16]), op=ALU.add),
                       reads=[V16], appends=[BIG1])
                for h in range(8):
                    cd = BIG1.t[:, h * 256:(h + 1) * 256]
                    ba = B16.t[:, h * 16:h * 16 + 8]
                    bb = B16.t[:, h * 16 + 8:h * 16 + 16]
                    op("dve", lambda e, cd=cd, ba=ba: e.max(out=ba, in_=cd), reads=[BIG1], appends=[B16])
                    op("dve", lambda e, cd=cd, ba=ba: e.match_replace(out=SCR.t[:], in_to_replace=ba, in_values=cd, imm_value=-1e30),
                       reads=[BIG1, B16], writes=[SCR])
                    op("dve", lambda e, bb=bb: e.max(out=bb, in_=SCR.t[:]), reads=[SCR], appends=[B16])
                    op("dve", lambda e, cd=cd, ba=ba, h=h: e.max_index(out=P16.t[:, h * 16:h * 16 + 8], in_max=ba, in_values=cd), reads=[BIG1, B16], appends=[P16])
                    op("dve", lambda e, cd=cd, bb=bb, h=h: e.max_index(out=P16.t[:, h * 16 + 8:h * 16 + 16], in_max=bb, in_values=cd), reads=[BIG1, B16], appends=[P16])
                b3 = B16.t[:].rearrange("p (h k) -> p h k", h=8)
                op("dve", lambda e: e.tensor_scalar(out=negm.t[:], in0=b3[:, :, 0], scalar1=-1.0, scalar2=None, op0=ALU.mult), reads=[B16], writes=[negm])
                for h in range(8):
                    op("act", lambda e, h=h: e.activation(out=E16.t[:, h * 16:(h + 1) * 16], in_=B16.t[:, h * 16:(h + 1) * 16], func=AF.Exp,
                                                          bias=negm.t[:, h:h + 1], scale=1.0, accum_out=Z.t[:, h:h + 1]),
                       reads=[B16, negm], appends=[E16, Z])
                op("dve", lambda e: e.reciprocal(out=rZ.t[:], in_=Z.t[:]), reads=[Z], writes=[rZ])
                op("dve", lambda e: e.tensor_tensor(out=E16.t[:].rearrange("p (h k) -> p h k", h=8), in0=E16.t[:].rearrange("p (h k) -> p h k", h=8),
                                                    in1=rZ.t[:].unsqueeze(2).to_broadcast([128, 8, 16]), op=ALU.mult),
                   reads=[E16, rZ], writes=[E16])
                op("dve", lambda e: e.tensor_copy(out=posf.t[:], in_=P16.t[:]), reads=[P16], writes=[posf])
                big2_3 = BIG2.t[:].rearrange("p (a b) -> p a b", b=16)
                big1_3 = BIG1.t[:].rearrange("p (a b) -> p a b", b=16)
                op("dve", lambda e: e.tensor_tensor(out=big2_3, in0=posf.t[:].unsqueeze(2).to_broadcast([128, 128, 16]),
                                                    in1=thrA.unsqueeze(1).to_broadcast([128, 128, 16]), op=ALU.is_ge),
                   reads=[posf, cst], writes=[BIG2])
                op("dve", lambda e: e.tensor_reduce(out=k1f.t[:], in_=big2_3, axis=AX.X, op=ALU.add), reads=[BIG2], writes=[k1f])
                op("dve", lambda e: e.scalar_tensor_tensor(out=k2f.t[:], in0=k1f.t[:], scalar=-16.0, in1=posf.t[:], op0=ALU.mult, op1=ALU.add),
                   reads=[k1f, posf], writes=[k2f])
                i3 = I16f.t[:].rearrange("p (h q k) -> p h q k", h=8, q=2)
                for half_, (kf, isel) in enumerate(((k1f, i1s), (k2f, i2s))):
                    op("dve", lambda e, kf=kf: e.tensor_tensor(out=big1_3, in0=iotaA.unsqueeze(1).to_broadcast([128, 128, 16]),
                                                              in1=kf.t[:].unsqueeze(2).to_broadcast([128, 128, 16]), op=ALU.is_equal),
                       reads=[kf, cst], writes=[BIG1])
                    for h in range(8):
                        op("dve", lambda e, h=h, half_=half_: e.tensor_tensor(out=big2_3[:, h * 16:(h + 1) * 16, :], in0=big1_3[:, h * 16:(h + 1) * 16, :],
                                                                               in1=i3[:, h, half_, :].unsqueeze(1).to_broadcast([128, 16, 16]), op=ALU.mult),
                           reads=[BIG1, I16f], appends=[BIG2])
                    op("dve", lambda e, isel=isel: e.tensor_reduce(out=isel.t[:], in_=big2_3, axis=AX.X, op=ALU.add), reads=[BIG2], writes=[isel])
                op("dve", lambda e: e.scalar_tensor_tensor(out=idxf.t[:], in0=i1s.t[:], scalar=128.0, in1=i2s.t[:], op0=ALU.mult, op1=ALU.add),
                   reads=[i1s, i2s], writes=[idxf])
                op("dve", lambda e: e.tensor_copy(out=IDX.t[:], in_=idxf.t[:]), reads=[idxf], writes=[IDX])
                if STAGE == 4:
                    op('dve', lambda e: e.tensor_copy(out=ACC.t[:, 0:128], in_=idxf.t[:]), reads=[idxf], writes=[ACC])
                    op('dve', lambda e: e.tensor_copy(out=ACC.t[:, 128:256], in_=E16.t[:]), reads=[E16], appends=[ACC])
                    op('dve', lambda e: e.tensor_copy(out=ACC.t[:, 256:384], in_=B16.t[:]), reads=[B16], appends=[ACC])
                    op('dve', lambda e: e.tensor_copy(out=ACC.t[:, 384:512], in_=H2g.t[:, 0:128]), reads=[H2g], appends=[ACC])
                    op('dve', lambda e: e.tensor_copy(out=ACC.t[:, 512:768], in_=V16.t[:]), reads=[V16], appends=[ACC])
                    op('dve', lambda e: e.tensor_copy(out=ACC.t[:, 768:1024], in_=qTp.t[:, 0:256]), reads=[qTp], appends=[ACC])
                    dump(ACC, n); n += 1; continue
                bacc = [banks[5], banks[6]]
                for g in range(32):
                    for kk in range(4):
                        k = g * 4 + kk
                        sl = GSb[k % 8]
                        dma("pool", lambda e, sl=sl, k=k: e.indirect_dma_start(out=sl.t[:], out_offset=None, in_=uvb_d,
                                                                               in_offset=bass.IndirectOffsetOnAxis(ap=IDX.t[:, k:k + 1], axis=0), bounds_check=breg(e), oob_is_err=False),
                            sl, reads=[IDX] + parts)
                        jb = junkD if k % 2 == 0 else junk
                        op("dve", lambda e, sl=sl, k=k, jb=jb: e.scalar_tensor_tensor(out=jb.t[:], in0=sl.t[:, 0:D], scalar=1.0, in1=H2g.t[:], op0=ALU.mult, op1=ALU.mult,
                                                                                      accum_out=ACTV.t[:, k:k + 1]),
                           reads=[sl, H2g], writes=[jb], appends=[ACTV])
                    op("act", lambda e, g=g: e.activation(out=GL.t[:, g * 4:(g + 1) * 4], in_=ACTV.t[:, g * 4:(g + 1) * 4], func=AF.Gelu), reads=[ACTV], appends=[GL])
                    for kk in range(4):
                        k = g * 4 + kk
                        sl = GSb[k % 8]
                        dg = DG[k % 4]
                        op("dve", lambda e, dg=dg, k=k: e.tensor_scalar(out=dg.t[:], in0=ident.t[:], scalar1=GL.t[:, k:k + 1], scalar2=E16.t[:, k:k + 1],
                                                                        op0=ALU.mult, op1=ALU.mult),
                           reads=[ident, GL, E16], writes=[dg])
                        for nbk in range(2):
                            op("pe", lambda e, dg=dg, sl=sl, nbk=nbk, k=k: e.matmul(bacc[nbk].t[:], lhsT=dg.t[:], rhs=sl.t[:, D + nbk * 512:D + (nbk + 1) * 512],
                                                                                     start=(k == 0), stop=(k == 127)),
                               reads=[dg, sl], appends=[bacc[nbk]])
                for nbk in range(2):
                    op("dve", lambda e, nbk=nbk: e.tensor_tensor(out=ACC.t[:, nbk * 512:(nbk + 1) * 512], in0=bacc[nbk].t[:],
                                                                 in1=X2.t[:, nbk * 512:(nbk + 1) * 512], op=ALU.add),
                       reads=[bacc[nbk], X2], appends=[ACC])
                r3 = rmsnorm_stats(ACC, 6, None)
                op("dve", lambda e, r3=r3: e.scalar_tensor_tensor(out=X2.t[:], in0=ACC.t[:], scalar=r3, in1=gfin_bc.t[:], op0=ALU.mult, op1=ALU.mult),
                   reads=[ACC, ss, gfin_bc], writes=[X2])
                dma("pool", lambda e, n=n: e.dma_start(out=out_d[n * 128:(n + 1) * 128, :], in_=X2.t[:]), outb, reads=[X2], append=True)
                n += 1
        S.wait_all("pool", [outb])
        S.emit()
    return nc


def _host_consts():
    f = np.float32
    ident = np.eye(128, dtype=f)
    s = np.arange(128)[:, None]
    t = np.arange(128)[None, :]
    hmask = ((s <= t) & ((s // 64) == (t // 64))).astype(f)
    reset = np.ones((128, 512), f)
    reset[:, ::64] = 0.0
    iota = np.tile(np.arange(16, dtype=f)[None, :], (128, 1))
    thr = np.tile(np.array([16 * (i + 1) for i in range(15)] + [1e9], dtype=f)[None, :], (128, 1))
    cst = np.zeros((128, 1024), f)
    cst[:, 0:128] = ident
    cst[:, 128:256] = hmask
    cst[:, 256:768] = reset
    cst[:, 768:784] = iota
    cst[:, 784:800] = thr
    k = np.arange(128, dtype=np.float64)[:, None]
    q = np.arange(128, dtype=np.float64)[None, :]
    tab = np.zeros((128, 4, 4, 128), np.float64)
    for b in range(4):
        for hl in range(2):
            hh = b if hl == 0 else 4 + b
            slope = 2.0 ** (-(hh + 1))
            tab[:, b, 0 * 2 + hl, :] = np.where(k > q, np.exp(-slope * (128 + q - k)), 0.0)
            tab[:, b, 1 * 2 + hl, :] = np.where(k <= q, np.exp(-slope * (q - k)), 0.0)
    return cst, tab.reshape(128, 2048).astype(f)


def _prep_shared(norm_mix_g, w_in, attn_sinks, hgrn_lb_logits, hgrn_norm_g, w_attn_proj, w_hgrn_proj, w_out,
                 norm_ffn_g, w_peer_q, peer_keys, peer_u, peer_v, norm_final_g):
    f = np.float32
    w_in = np.asarray(w_in[0], f)
    perm = []
    for m in range(4):
        perm += list(range(m * 64, (m + 1) * 64)) + list(range((4 + m) * 64, (5 + m) * 64))
    cols = perm + list(range(512, 4864))
    w_in_p = w_in[:, cols]
    wr = w_in_p.reshape(8, 128, 4864).transpose(1, 0, 2)
    wall = np.zeros((NPIECE, 128, 4096), f)
    bounds = [(0, 512), (512, 768), (768, 1280), (1280, 1792), (1792, 2304), (2304, 2816),
              (2816, 3328), (3328, 3840), (3840, 4352), (4352, 4864)]
    for g, (a, b) in enumerate(bounds):
        blk = np.zeros((128, 8, 512), f)
        blk[:, :, :b - a] = wr[:, :, a:b]
        wall[g] = blk.reshape(128, 4096)
    wap = np.asarray(w_attn_proj[0], f).reshape(8, 64, 1024).transpose(1, 0, 2)
    for half in range(2):
        blk = np.zeros((128, 8, 512), f)
        blk[0:64] = wap[:, :, half * 512:(half + 1) * 512]
        wall[10 + half] = blk.reshape(128, 4096)
    whp = np.asarray(w_hgrn_proj[0], f).reshape(4, 128, 1024).transpose(1, 0, 2)
    wall[12] = whp.reshape(128, 4096)
    wo = np.asarray(w_out[0], f).reshape(8, 128, 1024).transpose(1, 0, 2)
    for half in range(2):
        wall[13 + half] = np.ascontiguousarray(wo[:, :, half * 512:(half + 1) * 512]).reshape(128, 4096)
    wq = np.asarray(w_peer_q[0], f).reshape(8, 128, 2048).transpose(1, 0, 2)
    for pq in range(4):
        wall[15 + pq] = np.ascontiguousarray(wq[:, :, pq * 512:(pq + 1) * 512]).reshape(128, 4096)
    keys = np.asarray(peer_keys[0], f)
    keysT = keys.transpose(3, 1, 0, 2).reshape(128, 2048)
    gcols = np.zeros((128, 32), f)
    gcols[:, 0:8] = np.asarray(norm_mix_g[0], f).reshape(8, 128).T
    gcols[:, 8:16] = np.asarray(norm_ffn_g[0], f).reshape(8, 128).T
    gcols[:, 16:20] = np.asarray(hgrn_lb_logits[0], f).reshape(4, 128).T
    gcols[:, 20:24] = np.asarray(hgrn_lb_logits[1], f).reshape(4, 128).T
    gcols[:, 24] = np.asarray(hgrn_norm_g[0], f)
    cst, tab = _host_consts()
    return {
        "wall": wall,
        "pu": np.ascontiguousarray(np.asarray(peer_u[0], f)),
        "pv": np.ascontiguousarray(np.asarray(peer_v[0], f)),
        "keysT": np.ascontiguousarray(keysT),
        "gcols": gcols,
        "sinks": np.asarray(attn_sinks, f).reshape(1, 8),
        "gffn_bc": np.ascontiguousarray(np.tile(np.asarray(norm_ffn_g[0], f)[None, :], (128, 1))),
        "gfin_bc": np.ascontiguousarray(np.tile(np.asarray(norm_final_g, f)[None, :], (128, 1))),
        "attn_tab": tab,
        "consts": cst,
    }


def run(x, params, nseq, nt):
    shared = _prep_shared(**params)
    nc = build_nc(nseq, nt)
    xs = np.ascontiguousarray(np.asarray(x, np.float32)).reshape(NCORES, nseq * nt * 128, D)
    in_maps = [dict(shared, x=xs[i]) for i in range(NCORES)]
    res = run_bass_kernel_spmd(nc, in_maps, core_ids=list(range(NCORES)))
    out = np.stack([r["out"] for r in res.results], axis=0)
    return out.reshape(NCORES * nseq, nt * 128, D).astype(np.float32)


def kernel(x, norm_mix_g, w_in, attn_sinks, hgrn_lb_logits, hgrn_norm_g, w_attn_proj, w_hgrn_proj, w_out,
           norm_ffn_g, w_peer_q, peer_keys, peer_u, peer_v, norm_final_g):
    params = dict(norm_mix_g=norm_mix_g, w_in=w_in, attn_sinks=attn_sinks, hgrn_lb_logits=hgrn_lb_logits,
                  hgrn_norm_g=hgrn_norm_g, w_attn_proj=w_attn_proj, w_hgrn_proj=w_hgrn_proj, w_out=w_out,
                  norm_ffn_g=norm_ffn_g, w_peer_q=w_peer_q, peer_keys=peer_keys, peer_u=peer_u, peer_v=peer_v,
                  norm_final_g=norm_final_g)
    x = np.asarray(x)
    B = x.shape[0]
    return run(x, params, B // NCORES, x.shape[1] // 128)
```
